# Optimizing a Trainium2 kernel written in Bass

```python
import math
import jax, jax.numpy as jnp
from jax import lax
import numpy as np

D_MODEL = 1024
BATCH = 16
SEQ = 2048
DEPTH = 4

HEAD_DIM = 64
RMS_EPS = 1e-6
RWKV_HEADS = 4
RWKV_WIDTH = RWKV_HEADS * HEAD_DIM
RWKV_DECAY_RANK = 64
RWKV_ICLR_RANK = 64
RWKV_GATE_RANK = 128
RWKV_GN_EPS = 64e-5
GDN_HEADS = 4
GDN_WIDTH = GDN_HEADS * HEAD_DIM
GDN_CONV = 4
GDN_CHUNK = 64
ATTN_Q_HEADS = 8
ATTN_KV_HEADS = 2
ATTN_WIDTH = ATTN_Q_HEADS * HEAD_DIM
ATTN_KV_WIDTH = ATTN_KV_HEADS * HEAD_DIM
WINDOW = 128
ATTN_BLOCK = 128
NUM_BUCKETS = 32
MAX_DISTANCE = 128
D_MIX = RWKV_WIDTH + GDN_WIDTH + ATTN_WIDTH
D_FF = 4 * D_MODEL
RWKV_IN = 3 * RWKV_WIDTH + RWKV_DECAY_RANK + RWKV_ICLR_RANK + RWKV_GATE_RANK
GDN_IN = 4 * GDN_WIDTH + 2 * GDN_HEADS
ATTN_IN = ATTN_WIDTH + 2 * ATTN_KV_WIDTH
D_IN = RWKV_IN + GDN_IN + ATTN_IN

kernel_name = "hymba_style_rwkv7_gdn_swa_hybrid"


def split_cols(z, sizes):
    offs = [int(o) for o in np.cumsum(sizes)[:-1]]
    return jnp.split(z, offs, axis=-1)


def rms_norm(x, w, eps=RMS_EPS):
    xf = x.astype(jnp.float32)
    y = xf * lax.rsqrt(jnp.mean(xf * xf, axis=-1, keepdims=True) + eps)
    return (y * w).astype(x.dtype)


def l2_normalize(x, eps=1e-6):
    return x * lax.rsqrt(jnp.sum(x * x, axis=-1, keepdims=True) + eps)


def token_shift(z):
    return jnp.pad(z, ((0, 0), (1, 0), (0, 0)))[:, :-1]


def causal_depthwise_conv(x, w):
    K, C = w.shape
    return lax.conv_general_dilated(x, w.astype(x.dtype)[:, None, :], window_strides=(1,),
                                    padding=[(K - 1, 0)], dimension_numbers=('NWC', 'WIO', 'NWC'),
                                    feature_group_count=C)


def rwkv7_recurrence(r, w, k, v, a, b):
    Bsz, S, H, N = r.shape

    def step(state, inp):
        r_t, w_t, k_t, v_t, a_t, b_t = inp
        sa = jnp.einsum('bhij,bhj->bhi', state, a_t)
        state = (state * w_t[:, :, None, :] + sa[..., None] * b_t[:, :, None, :]
                 + v_t[..., None] * k_t[:, :, None, :])
        return state, jnp.einsum('bhij,bhj->bhi', state, r_t)

    xs = tuple(jnp.moveaxis(t, 1, 0) for t in (r, w, k, v, a, b))
    state0 = jnp.zeros((Bsz, H, N, N), jnp.float32)
    _, y = lax.scan(step, state0, xs)
    return jnp.moveaxis(y, 0, 1)


def rwkv7_mixer(z, mu, w0, w_up, a0, a_up, g_up, k_k, k_a, r_k, lnx_w, lnx_b):
    Bsz, S, _ = z.shape
    H, N = RWKV_HEADS, HEAD_DIM
    z = z + (token_shift(z) - z) * mu
    r, k, v, dw, da, dg = split_cols(z, (RWKV_WIDTH, RWKV_WIDTH, RWKV_WIDTH,
                                         RWKV_DECAY_RANK, RWKV_ICLR_RANK, RWKV_GATE_RANK))
    log_w = -jax.nn.softplus(-(w0 + jnp.tanh(dw) @ w_up)) - 0.5
    decay = jnp.exp(-jnp.exp(log_w))
    iclr = jax.nn.sigmoid(a0 + da @ a_up)
    gate = jax.nn.sigmoid(dg) @ g_up
    heads = lambda t: t.reshape(Bsz, S, H, N)
    kk = l2_normalize(heads(k * k_k))
    k = k * (1.0 + (iclr - 1.0) * k_a)
    r_h, k_h, v_h = heads(r), heads(k), heads(v)
    y = rwkv7_recurrence(r_h, heads(decay), k_h, v_h, -kk, kk * heads(iclr))
    mean = jnp.mean(y, axis=-1, keepdims=True)
    var = jnp.mean(jnp.square(y - mean), axis=-1, keepdims=True)
    y = ((y - mean) * lax.rsqrt(var + RWKV_GN_EPS)).reshape(Bsz, S, RWKV_WIDTH) * lnx_w + lnx_b
    bonus = jnp.sum(r_h * k_h * r_k, axis=-1, keepdims=True) * v_h
    return (y + bonus.reshape(Bsz, S, RWKV_WIDTH)) * gate


def gated_delta_rule_chunked(q, k, v, g, beta):
    Bsz, S, H, Dk = q.shape
    Dv = v.shape[-1]
    C = GDN_CHUNK
    N = S // C
    to_chunks = lambda t: jnp.moveaxis(t.reshape(Bsz, N, C, H, -1), 3, 1)
    q = to_chunks(q) * (Dk ** -0.5)
    k = to_chunks(k)
    v = to_chunks(v)
    g = jnp.cumsum(to_chunks(g[..., None])[..., 0], axis=-1)
    beta = to_chunks(beta[..., None])
    causal = jnp.tril(jnp.ones((C, C), dtype=bool))
    decay = jnp.exp(jnp.where(causal, g[..., :, None] - g[..., None, :], -jnp.inf))
    eye = jnp.eye(C, dtype=q.dtype)
    k_beta = k * beta
    strict = jnp.einsum('bhncd,bhnsd->bhncs', k_beta, k) * decay * (1.0 - eye)
    T = lax.linalg.triangular_solve(eye + strict, jnp.broadcast_to(eye, strict.shape),
                                    left_side=True, lower=True)
    u = T @ (v * beta)
    w = T @ (k_beta * jnp.exp(g)[..., None])
    intra = jnp.einsum('bhncd,bhnsd->bhncs', q, k) * decay
    q_dec = q * jnp.exp(g)[..., None]
    g_last = g[..., -1]
    k_dec = k * jnp.exp(g_last[..., None] - g)[..., None]

    def step(state, inp):
        q_i, k_i, u_i, w_i, a_i, gl_i = inp
        v_new = u_i - w_i @ state
        o = q_i @ state + a_i @ v_new
        state = state * jnp.exp(gl_i)[..., None, None] + jnp.swapaxes(k_i, -1, -2) @ v_new
        return state, o

    xs = tuple(jnp.moveaxis(t, 2, 0) for t in (q_dec, k_dec, u, w, intra, g_last))
    state0 = jnp.zeros((Bsz, H, Dk, Dv), jnp.float32)
    _, o = lax.scan(step, state0, xs)
    return jnp.transpose(o, (1, 0, 3, 2, 4)).reshape(Bsz, S, H, Dv)


def gdn_mixer(z, conv_w, a_log, dt_bias, norm_w):
    Bsz, S, _ = z.shape
    H, D = GDN_HEADS, HEAD_DIM
    qkv, gate, b, a = split_cols(z, (3 * GDN_WIDTH, GDN_WIDTH, H, H))
    qkv = jax.nn.silu(causal_depthwise_conv(qkv, conv_w))
    q, k, v = (t.reshape(Bsz, S, H, D) for t in jnp.split(qkv, 3, axis=-1))
    beta = jax.nn.sigmoid(b)
    g = -jnp.exp(a_log) * jax.nn.softplus(a + dt_bias)
    o = gated_delta_rule_chunked(l2_normalize(q), l2_normalize(k), v, g, beta)
    o = rms_norm(o, norm_w) * jax.nn.silu(gate.reshape(Bsz, S, H, D))
    return o.reshape(Bsz, S, GDN_WIDTH)


def t5_bucket(dist):
    max_exact = NUM_BUCKETS // 2
    nf = jnp.maximum(dist, max_exact).astype(jnp.float32)
    large = max_exact + (jnp.log(nf / max_exact) / math.log(MAX_DISTANCE / max_exact)
                         * (NUM_BUCKETS - max_exact)).astype(jnp.int32)
    large = jnp.minimum(large, NUM_BUCKETS - 1)
    return jnp.where(dist < max_exact, dist, large)


def band_offsets():
    L = ATTN_BLOCK
    i = jnp.arange(L)[:, None]
    j = jnp.arange(2 * L)[None, :]
    return i + L - j


def t5_band_bias(rel_bias):
    dist = jnp.maximum(band_offsets(), 0)
    return jnp.transpose(rel_bias[t5_bucket(dist)], (2, 0, 1))


def band_mask(nb):
    dist = band_offsets()
    in_window = (dist >= 0) & (dist < WINDOW)
    key_exists = (jnp.arange(nb)[:, None, None] > 0) | (jnp.arange(2 * ATTN_BLOCK)[None, None, :] >= ATTN_BLOCK)
    return in_window & key_exists


def swa_mixer(z, q_norm_w, k_norm_w, sinks, bias_band):
    Bsz, S, _ = z.shape
    Hq, Hkv, D, L = ATTN_Q_HEADS, ATTN_KV_HEADS, HEAD_DIM, ATTN_BLOCK
    G = Hq // Hkv
    nb = S // L
    q, k, v = split_cols(z, (ATTN_WIDTH, ATTN_KV_WIDTH, ATTN_KV_WIDTH))
    q = rms_norm(q.reshape(Bsz, S, Hq, D), q_norm_w).reshape(Bsz, nb, L, Hkv, G, D)
    k = rms_norm(k.reshape(Bsz, S, Hkv, D), k_norm_w)
    v = v.reshape(Bsz, S, Hkv, D)

    def band(t):
        tb = jnp.pad(t, ((0, 0), (L, 0), (0, 0), (0, 0))).reshape(Bsz, nb + 1, L, Hkv, D)
        return jnp.concatenate([tb[:, :-1], tb[:, 1:]], axis=2)

    scores = (jnp.einsum('bnqhgd,bnkhd->bhgnqk', q, band(k)) * (D ** -0.5)
              + bias_band.reshape(Hkv, G, 1, L, 2 * L).astype(jnp.float32))
    scores = jnp.where(band_mask(nb), scores, -jnp.inf)
    sink = jnp.broadcast_to(sinks.astype(jnp.float32).reshape(1, Hkv, G, 1, 1, 1), scores.shape[:-1] + (1,))
    probs = jax.nn.softmax(jnp.concatenate([scores, sink], axis=-1), axis=-1)[..., :-1]
    out = jnp.einsum('bhgnqk,bnkhd->bnqhgd', probs, band(v))
    return out.reshape(Bsz, S, ATTN_WIDTH)


def setup_inputs(seed: int = 0) -> dict:
    key = jax.random.key(seed)
    ks = iter(jax.random.split(key, 32))
    nrm = lambda shape, scale: scale * jax.random.normal(next(ks), shape, jnp.float32)
    L = DEPTH
    dt = jnp.exp(jax.random.uniform(next(ks), (L, GDN_HEADS), jnp.float32,
                                    minval=math.log(1e-3), maxval=math.log(1e-1)))
    return {
        "x": nrm((BATCH, SEQ, D_MODEL), 1.0),
        "ln1_w": 1.0 + nrm((L, D_MODEL), 0.1),
        "w_in": nrm((L, D_MODEL, D_IN), D_MODEL ** -0.5),
        "rwkv_mu": jax.random.uniform(next(ks), (L, RWKV_IN), jnp.float32),
        "rwkv_w0": jax.random.uniform(next(ks), (L, RWKV_WIDTH), jnp.float32, minval=-6.0, maxval=-1.0),
        "rwkv_w_up": nrm((L, RWKV_DECAY_RANK, RWKV_WIDTH), 0.1),
        "rwkv_a0": nrm((L, RWKV_WIDTH), 0.1),
        "rwkv_a_up": nrm((L, RWKV_ICLR_RANK, RWKV_WIDTH), RWKV_ICLR_RANK ** -0.5),
        "rwkv_g_up": nrm((L, RWKV_GATE_RANK, RWKV_WIDTH), RWKV_GATE_RANK ** -0.5),
        "rwkv_k_k": 0.85 + nrm((L, RWKV_WIDTH), 0.05),
        "rwkv_k_a": 1.0 + nrm((L, RWKV_WIDTH), 0.05),
        "rwkv_r_k": nrm((L, RWKV_HEADS, HEAD_DIM), 0.1),
        "rwkv_lnx_w": 1.0 + nrm((L, RWKV_WIDTH), 0.1),
        "rwkv_lnx_b": nrm((L, RWKV_WIDTH), 0.02),
        "gdn_conv_w": nrm((L, GDN_CONV, 3 * GDN_WIDTH), GDN_CONV ** -0.5),
        "gdn_a_log": jnp.log(jax.random.uniform(next(ks), (L, GDN_HEADS), jnp.float32, minval=1.0, maxval=16.0)),
        "gdn_dt_bias": dt + jnp.log(-jnp.expm1(-dt)),
        "gdn_norm_w": 1.0 + nrm((L, HEAD_DIM), 0.1),
        "attn_q_norm_w": 1.0 + nrm((L, HEAD_DIM), 0.1),
        "attn_k_norm_w": 1.0 + nrm((L, HEAD_DIM), 0.1),
        "attn_sinks": nrm((L, ATTN_Q_HEADS), 1.0),
        "rel_bias": nrm((NUM_BUCKETS, ATTN_Q_HEADS), 0.5),
        "w_out": nrm((L, D_MIX, D_MODEL), D_MIX ** -0.5),
        "ln2_w": 1.0 + nrm((L, D_MODEL), 0.1),
        "w_ff1": nrm((L, D_MODEL, D_FF), D_MODEL ** -0.5),
        "w_ff2": nrm((L, D_FF, D_MODEL), 0.5 * D_FF ** -0.5),
    }


def reference(x, ln1_w, w_in, rwkv_mu, rwkv_w0, rwkv_w_up, rwkv_a0, rwkv_a_up, rwkv_g_up,
              rwkv_k_k, rwkv_k_a, rwkv_r_k, rwkv_lnx_w, rwkv_lnx_b, gdn_conv_w, gdn_a_log,
              gdn_dt_bias, gdn_norm_w, attn_q_norm_w, attn_k_norm_w, attn_sinks, rel_bias,
              w_out, ln2_w, w_ff1, w_ff2):
    bias_band = t5_band_bias(rel_bias)
    for l in range(DEPTH):
        h = rms_norm(x, ln1_w[l])
        z = (h @ w_in[l]).astype(jnp.float32)
        z_a, z_b, z_c = split_cols(z, (RWKV_IN, GDN_IN, ATTN_IN))
        y_a = rwkv7_mixer(z_a, rwkv_mu[l], rwkv_w0[l], rwkv_w_up[l], rwkv_a0[l], rwkv_a_up[l],
                          rwkv_g_up[l], rwkv_k_k[l], rwkv_k_a[l], rwkv_r_k[l], rwkv_lnx_w[l], rwkv_lnx_b[l])
        y_b = gdn_mixer(z_b, gdn_conv_w[l], gdn_a_log[l], gdn_dt_bias[l], gdn_norm_w[l])
        y_c = swa_mixer(z_c, attn_q_norm_w[l], attn_k_norm_w[l], attn_sinks[l], bias_band)
        y = jnp.concatenate([y_a, y_b, y_c], axis=-1).astype(x.dtype)
        x = x + y @ w_out[l]
        h = rms_norm(x, ln2_w[l])
        x = x + jnp.square(jax.nn.relu(h @ w_ff1[l])) @ w_ff2[l]
    return x
```

```python
import contextlib
import os
CUT = int(os.environ.get('CUT', '99'))
ALLINC = int(os.environ.get('ALLINC', '0'))
import numpy as np
import concourse.bass as bass
import concourse.mybir as mybir
from concourse.bass_utils import run_bass_kernel_spmd

F32 = mybir.dt.float32
BF16 = mybir.dt.bfloat16
AF = mybir.ActivationFunctionType
ALU = mybir.AluOpType

NCORES = 8
SEQ = 2048
NSEQ = 2
NTOK = NSEQ * SEQ
D = 1024
DIN = 2824
DFF = 4096
TT = 512
NTILE = NTOK // TT
TPS = SEQ // TT
CH = 64
NCH = TT // CH
NEG = -30000.0
DECAY_C = -0.6065306597126334

C_ID = 0
C_B1 = 128
C_B64 = 256
C_OD = 384
C_RST = 512
C_MT4 = 1024
C_MSL4 = 2048
C_I64 = 2304
C_SEL = 2560
C_BM = 2816
C_MNI = 3072
NCST = 3136
P_LN1, P_LN2, P_MU, P_W0, P_A0, P_KK, P_KA, P_RK, P_LXW, P_LXB, P_CONV = 0, 8, 16, 24, 26, 28, 30, 32, 34, 36, 38
P_GNW, P_QNW, P_KNW, P_ALOG, P_DTB, P_SINK = 62, 63, 64, 65, 66, 67
NPP = 75


class Buf:
    __slots__ = ("name", "w", "r", "pe_rows")

    def __init__(self, name):
        self.name = name
        self.w = None
        self.r = {}
        self.pe_rows = None


class View:
    __slots__ = ("ap", "keys")

    def __init__(self, ap, keys):
        self.ap = ap
        self.keys = keys

    def __getitem__(self, idx):
        return View(self.ap[idx], self.keys)

    def rr(self, pat, **kw):
        return View(self.ap.rearrange(pat, **kw), self.keys)


class Tl:
    def __init__(self, h, key):
        self.h = h
        self.key = key

    def __getitem__(self, idx):
        return View(self.h[idx], (self.key,))


def dv(ap, *keys):
    return View(ap, tuple(keys))


class Prog:
    COMPUTE = ("pe", "act", "dve", "pool")

    def __init__(self, nc, ndma=14):
        self.nc = nc
        self.E = {"pe": nc.tensor, "act": nc.scalar, "dve": nc.vector, "pool": nc.gpsimd, "sp": nc.sync}
        self.sem = {e: nc.alloc_semaphore("c_" + e) for e in self.COMPUTE}
        self.cnt = {e: 0 for e in self.COMPUTE}
        self.known = {e: {} for e in self.E}
        self.dsem = {}
        for q in ("sp", "pool"):
            self.dsem[q] = [[nc.alloc_semaphore(f"d_{q}{i}"), 0] for i in range(ndma)]
        self.drr = {q: 0 for q in self.dsem}
        self.bufs = {}
        self.nins = 0
        self.log = {e: [] for e in self.E}

    def buf(self, key):
        b = self.bufs.get(key)
        if b is None:
            b = self.bufs[key] = Buf(key)
        return b

    def _need(self, eng, ev, waits):
        if ev is None:
            return
        sem, val = ev
        if self.known[eng].get(sem, 0) >= val:
            return
        if val > waits.get(sem, 0):
            waits[sem] = val

    def _deps(self, eng, reads, writes, is_dma):
        waits = {}
        mysem = None if is_dma else self.sem[eng]
        for b in reads:
            self._need(eng, b.w, waits)
        for b in writes:
            if b.w is not None and (is_dma or b.w[0] is not mysem):
                self._need(eng, b.w, waits)
            for s, v in b.r.items():
                if is_dma or s is not mysem:
                    self._need(eng, (s, v), waits)
        return waits

    def _commit(self, ev, reads, writes):
        s, v = ev
        for b in reads:
            if b.r.get(s, 0) < v:
                b.r[s] = v
        for b in writes:
            b.w = ev
            b.r = {}

    def _bl(self, views):
        out = []
        for v in views:
            if v is None or not isinstance(v, View):
                continue
            for k in v.keys:
                out.append(self.buf(k))
        return out

    def op(self, eng, fn, reads=(), writes=(), inc=True, extra=()):
        if ALLINC or eng == "pe":
            inc = True
        reads = self._bl(reads)
        writes = self._bl(writes)
        for b in reads:
            if isinstance(b.name, str) and b.name.startswith("pb") and b not in writes:
                writes.append(b)
        waits = self._deps(eng, reads, writes, False)
        for ev in extra:
            self._need(eng, ev, waits)
        e = self.E[eng]
        for s, v in waits.items():
            self.known[eng][s] = v
            e.wait_ge(s, v)
            self.nins += 1
        sem = self.sem[eng]
        ins = fn(e)
        self.nins += 1
        self.log[eng].append((list(waits.items()), (sem, 1) if inc else None))
        if inc:
            self.cnt[eng] += 1
            ev = (sem, self.cnt[eng])
            ins.then_inc(sem, 1)
        else:
            ev = (sem, self.cnt[eng] + 1)
        self._commit(ev, reads, writes)

    def dma(self, q, out, in_):
        reads = self._bl([in_])
        writes = self._bl([out])
        waits = self._deps(q, reads, writes, True)
        pool = self.dsem[q]
        i = self.drr[q]
        self.drr[q] = (i + 1) % len(pool)
        sem, n = pool[i]
        if n > 0:
            self._need(q, (sem, 16 * n), waits)
        pool[i][1] = n + 1
        ev = (sem, 16 * (n + 1))
        e = self.E[q]
        for s, v in waits.items():
            self.known[q][s] = v
            e.wait_ge(s, v)
            self.nins += 1
        e.dma_start(out=out.ap, in_=in_.ap).then_inc(sem, 16)
        self.nins += 1
        self.log[q].append((list(waits.items()), (sem, 16)))
        self._commit(ev, reads, writes)

    def simulate(self):
        pos = {e: 0 for e in self.log}
        val = {}
        prog = True
        while prog:
            prog = False
            for e, lst in self.log.items():
                while pos[e] < len(lst):
                    waits, inc = lst[pos[e]]
                    if all(val.get(s_, 0) >= v for s_, v in waits):
                        if inc is not None:
                            val[inc[0]] = val.get(inc[0], 0) + inc[1]
                        pos[e] += 1
                        prog = True
                    else:
                        break
        stuck = {e: (pos[e], len(l)) for e, l in self.log.items() if pos[e] < len(l)}
        for e in stuck:
            waits, inc = self.log[e][pos[e]]
            print("STUCK", e, pos[e], [(s_.name, v, val.get(s_, 0)) for s_, v in waits])
        return stuck

    def barrier(self):
        targets = [(self.sem[e], self.cnt[e]) for e in self.COMPUTE if self.cnt[e] > 0]
        for q in self.dsem:
            targets += [(s_, 16 * n) for s_, n in self.dsem[q] if n > 0]
        for eng, e in self.E.items():
            wl = []
            for s_, v in targets:
                if self.known[eng].get(s_, 0) < v:
                    self.known[eng][s_] = v
                    e.wait_ge(s_, v)
                    wl.append((s_, v))
                    self.nins += 1
            self.log[eng].append((wl, None))

    def wait_all_dma(self, q):
        e = self.E[q]
        for s, n in self.dsem[q]:
            if n > 0:
                e.wait_ge(s, 16 * n)

    def _pe_rows(self, out, lhsT):
        b0 = lhsT.ap.base_partition()
        k = lhsT.ap.partition_size()
        rows = (b0 // 32, (b0 + k + 31) // 32)
        extra = []
        for key in out.keys:
            b = self.buf(key)
            pr = b.pe_rows
            if pr is not None and (pr[1] <= rows[0] or rows[1] <= pr[0]) and b.w is not None and b.w[0] is self.sem["pe"]:
                extra.append(b.w)
            b.pe_rows = rows
        return extra

    def mm(self, out, lhsT, rhs, start=True, stop=True, inc=True):
        extra = self._pe_rows(out, lhsT)
        self.op("pe", lambda e: e.matmul(out.ap, lhsT.ap, rhs.ap, start=start, stop=stop),
                reads=[lhsT, rhs], writes=[out], inc=inc, extra=extra)

    def tr(self, out, in_, ident, inc=True):
        extra = self._pe_rows(out, in_)
        if os.environ.get("TRMODE", "tr") == "mm":
            self.op("pe", lambda e: e.matmul(out.ap, in_.ap, ident.ap, start=True, stop=True), reads=[in_, ident], writes=[out], extra=extra)
        else:
            self.op("pe", lambda e: e.transpose(out.ap, in_.ap, ident.ap), reads=[in_, ident], writes=[out], extra=extra)

    def act(self, out, in_, func, bias=None, scale=None, eng="act"):
        kw = {}
        rd = [in_]
        if bias is not None:
            if isinstance(bias, View):
                kw["bias"] = bias.ap
                rd.append(bias)
            else:
                kw["bias"] = bias
        if scale is not None:
            if isinstance(scale, View):
                kw["scale"] = scale.ap
                rd.append(scale)
            else:
                kw["scale"] = scale
        self.op("act", lambda e: e.activation(out.ap, in_.ap, func, **kw), reads=rd, writes=[out])

    def tt(self, eng, out, a, b, op):
        self.op(eng, lambda e: e.tensor_tensor(out.ap, a.ap, b.ap, op), reads=[a, b], writes=[out])

    def ts(self, eng, out, a, s1, op0, s2=None, op1=None):
        rd = [a]
        v1 = s1
        if isinstance(s1, View):
            rd.append(s1)
            v1 = s1.ap
        v2 = s2
        if isinstance(s2, View):
            rd.append(s2)
            v2 = s2.ap
        if op1 is None:
            self.op(eng, lambda e: e.tensor_scalar(out.ap, a.ap, v1, None, op0), reads=rd, writes=[out])
        else:
            self.op(eng, lambda e: e.tensor_scalar(out.ap, a.ap, v1, v2, op0, op1), reads=rd, writes=[out])

    def stt(self, eng, out, a, s, b, op0, op1):
        eng = "dve"
        rd = [a, b]
        sv = s
        if isinstance(s, View):
            rd.append(s)
            sv = s.ap
        self.op(eng, lambda e: e.scalar_tensor_tensor(out.ap, a.ap, sv, b.ap, op0, op1), reads=rd, writes=[out])

    def cp(self, eng, out, in_):
        if eng == "act":
            self.op("act", lambda e: e.copy(out.ap, in_.ap), reads=[in_], writes=[out])
        else:
            self.op(eng, lambda e: e.tensor_copy(out.ap, in_.ap), reads=[in_], writes=[out])

    def memset(self, eng, out, val):
        self.op(eng, lambda e: e.memset(out.ap, val), writes=[out])

    def recip(self, out, in_):
        self.op("dve", lambda e: e.reciprocal(out.ap, in_.ap), reads=[in_], writes=[out])

    def scan(self, out, d0, d1):
        self.op("dve", lambda e: e.tensor_tensor_scan(out.ap, d0.ap, d1.ap, 0.0, ALU.mult, ALU.add),
                reads=[d0, d1], writes=[out])


class Builder:
    def __init__(self, nlayers, debug=False, stop_after=None):
        self.stop_after = stop_after
        self.mix_parts = ("rwkv", "gdn", "attn")
        self.mix_tiles = TPS
        self.mix_nseq = NSEQ
        if stop_after and stop_after.startswith("mix:"):
            f = stop_after.split(":")
            self.mix_parts = tuple(f[1].split(","))
            self.mix_tiles = int(f[2])
            self.mix_nseq = int(f[3])
        self.L = nlayers
        nc = self.nc = bass.Bass("TRN2", target_bir_lowering=False)
        self.p = Prog(nc)
        L = nlayers
        di = lambda n, s, dt=F32: nc.dram_tensor(n, s, dt, kind="ExternalInput")
        self.xT = di("xT", [D, NTOK])
        self.w_in = di("w_in", [L, D, DIN])
        self.w_out = di("w_out", [L, D, D])
        self.w_ff1 = di("w_ff1", [L, D, DFF])
        self.w_ff2 = di("w_ff2", [L, DFF, D])
        self.w_up = di("w_up", [L, 64, 256])
        self.a_up = di("a_up", [L, 64, 256])
        self.g_up = di("g_up", [L, 128, 256])
        self.pp = di("pp", [L, 128, NPP])
        self.cst = di("cst", [128, NCST])
        self.bband = di("bband", [128, 2 * 8 * 128])
        self.outT = nc.dram_tensor("outT", [D, NTOK], F32, kind="ExternalOutput")
        self.xa = nc.dram_tensor("xa_d", [D, NTOK], F32)
        kd = "ExternalOutput" if debug else "Internal"
        self.xb = nc.dram_tensor("xb_d", [D, NTOK], F32, kind=kd)
        self.z_d = nc.dram_tensor("z_d", [DIN, NTOK], F32, kind=kd)
        self.vtm_d = nc.dram_tensor("vtm_d", [NTOK, 128], F32, kind=kd)
        self.y_d = nc.dram_tensor("y_d", [D, NTOK], BF16, kind=kd)
        self._uid = 0

    def sb(self, st, shape, dt=F32, name=None):
        self._uid += 1
        name = name or f"t{self._uid}"
        h = st.enter_context(self.nc.sbuf_tensor(f"{name}_{self._uid}", shape, dt))
        return Tl(h, f"{name}_{self._uid}")

    def ps(self, st, n):
        out = []
        for i in range(n):
            self._uid += 1
            h = st.enter_context(self.nc.psum_tensor(f"ps{self._uid}", [128, 512], F32))
            out.append(h)
        return out

    def build(self):
        p = self.p
        with contextlib.ExitStack() as st:
            self.cs = self.sb(st, [128, NCST], F32, "cst")
            p.dma("sp", self.cs[:], dv(self.cst.ap(), "cst_d"))
            self.ppt = self.sb(st, [128, NPP], F32, "pp")
            self.der = self.sb(st, [128, 12], F32, "der")
            cur = self.xT
            for l in range(self.L):
                p.dma("sp", self.ppt[:], dv(self.pp.ap()[l], "pp_d"))
                p.ts("dve", self.der[:, 0:1], self.ppt[:, P_QNW:P_QNW + 1], 0.125, ALU.mult)
                p.act(self.der[0:4, 1:2], self.ppt[0:4, P_ALOG:P_ALOG + 1], AF.Exp)
                p.ts("dve", self.der[0:4, 1:2], self.der[0:4, 1:2], -1.0, ALU.mult)
                p.act(self.der[:, 2:10], self.ppt[:, P_SINK:P_SINK + 8], AF.Exp)
                last = (l == self.L - 1)
                self.phase_inproj(l, cur)
                p.barrier()
                if self.stop_after == "inproj":
                    break
                self.phase_mix(l)
                p.barrier()
                if self.stop_after and self.stop_after.startswith("mix"):
                    break
                self.phase_outproj(l, cur, self.xb)
                p.barrier()
                if self.stop_after == "outproj":
                    break
                dst = self.outT if last else self.xa
                self.phase_ffn(l, self.xb, dst)
                p.barrier()
                cur = self.xa
            p.wait_all_dma("pool")
            p.wait_all_dma("sp")
        return self.nc

    def rms_tile(self, xt, ht, lncol, sq2, rstd, pbank, pkey):
        p = self.p
        ones = self.cs[:, C_OD:C_OD + 128]
        pv = dv(pbank[:, :], pkey)
        for c in range(8):
            s = sq2[c % 2]
            p.act(s[:], xt[:, c, :], AF.Square)
            p.mm(pv, ones, s[:], start=(c == 0), stop=(c == 7), inc=(c == 7) or True)
        p.act(rstd[:], pv, AF.Sqrt, bias=1e-6)
        p.recip(rstd[:], rstd[:])
        for c in range(8):
            eng = "dve" if c % 2 == 0 else "pool"
            p.stt(eng, ht[:, c, :], xt[:, c, :], self.ppt[:, lncol + c: lncol + c + 1], rstd[:], ALU.mult, ALU.mult)

    def phase_inproj(self, l, xsrc):
        p = self.p
        with contextlib.ExitStack() as st:
            wb = self.sb(st, [128, 8, DIN], BF16, "winb")
            for c in range(8):
                p.dma("pool", wb[:, c, :], dv(self.w_in.ap()[l, c * 128:(c + 1) * 128, :], "w_in_d"))
            xts = [self.sb(st, [128, 8, TT], F32, "xt") for _ in range(2)]
            ht = self.sb(st, [128, 8, TT], BF16, "ht")
            sq2 = [self.sb(st, [128, TT], F32, "sq") for _ in range(2)]
            rstd = self.sb(st, [128, TT], F32, "rstd")
            stg = [self.sb(st, [128, 2, TT], F32, "stg") for _ in range(3)]
            vst = self.sb(st, [128, 4, 128], F32, "vst")
            banks = self.ps(st, 8)
            xsrc_v = xsrc.ap().rearrange("(c p) t -> p c t", p=128)
            ngrp = 22
            for j in range(NTILE):
                xt = xts[j % 2]
                t0 = j * TT
                for hh in range(2):
                    p.dma("sp", xt[:, hh * 4:(hh + 1) * 4, :], dv(xsrc_v[:, hh * 4:(hh + 1) * 4, t0:t0 + TT], ("x", id(xsrc), j)))
                self.rms_tile(xt, ht, P_LN1, sq2, rstd, banks[7], "pb7")
                gi = 0
                for g in range(ngrp):
                    bk = banks[g % 6]
                    pv = dv(bk[:, :], f"pb{g % 6}")
                    for c in range(8):
                        p.mm(pv, wb[:, c, g * 128:(g + 1) * 128], ht[:, c, :], start=(c == 0), stop=(c == 7), inc=(c == 7))
                    s = stg[(g // 2) % 3]
                    if g % 2 == 0:
                        p.cp("act", s[:, 0, :], pv)
                    else:
                        p.cp("dve", s[:, 1, :], pv)
                    if g % 2 == 1 or g == ngrp - 1:
                        g0 = g - (g % 2)
                        ng = g - g0 + 1
                        dst = self.z_d.ap()[g0 * 128:(g0 + ng) * 128, t0:t0 + TT].rearrange("(g p) t -> p g t", p=128)
                        p.dma("pool", dv(dst, ("z", g0, j), ("z", g0 + 1, j)), s[:, 0:ng, :])
                pv = dv(banks[6][:, :], "pb6")
                for q in range(4):
                    for c in range(8):
                        p.mm(dv(banks[6][:, q * 128:(q + 1) * 128], "pb6"), ht[:, c, q * 128:(q + 1) * 128],
                             wb[:, c, 2696:2824], start=(c == 0), stop=(c == 7), inc=(c == 7))
                p.cp("act", vst[:].rr("p a b -> p (a b)"), pv)
                dst = self.vtm_d.ap()[t0:t0 + TT, :].rearrange("(q p) f -> p q f", p=128)
                p.dma("pool", dv(dst, ("vtm", j)), vst[:])

    def dplr_setup(self, st):
        s = {}
        s["tmV"] = self.sb(st, [64, NCH, 256], F32, "tmV")
        s["tmB"] = self.sb(st, [64, NCH, 256], F32, "tmB")
        s["tmK"] = self.sb(st, [64, NCH, 256], F32, "tmK")
        s["GTs"] = [self.sb(st, [64, 4, 256], F32, "GTs") for _ in range(2)]
        s["M"] = [self.sb(st, [64, 2, 256], F32, "M") for _ in range(2)]
        s["Tt"] = self.sb(st, [64, NCH, 256], F32, "Tt")
        s["DT"] = self.sb(st, [64, 256], F32, "DT")
        s["DTm"] = self.sb(st, [64, 1024], F32, "DTm")
        s["gcol"] = self.sb(st, [64, 32], F32, "gcol")
        s["Hs"] = [self.sb(st, [128, 64], F32, "Hs") for _ in range(2)]
        s["Xs"] = self.sb(st, [64, 256], F32, "Xs")
        s["Us"] = self.sb(st, [64, 256], F32, "Us")
        return s

    def dplr_tile(self, s, banks, arT, bT, kT, vT, pc, H, yout, arG=None, bG=None, kG=None, gam=None):
        p = self.p
        cs = self.cs
        ident = cs[:, C_ID:C_ID + 128]
        bk = lambda i: dv(banks[i][:, :], f"pb{i}")
        bks = lambda i, sl: dv(banks[i][sl], f"pb{i}")
        scalar_mode = gam is not None
        if arG is None:
            arG, bG, kG = arT, bT, kT
        if scalar_mode:
            for c in range(NCH):
                p.tr(bks(7, (slice(0, 64), slice(c * 4, c * 4 + 4))), gam[0:4, c * 64:(c + 1) * 64], cs[0:4, C_ID:C_ID + 4], inc=(c == NCH - 1))
            p.cp("act", s["gcol"][:], bks(7, (slice(0, 64), slice(0, 32))))
        for ti, (src, dst) in enumerate(((vT, s["tmV"]), (bT, s["tmB"]), (kT, s["tmK"]))):
            for cp2 in range(NCH // 2):
                b = 2 + (ti * (NCH // 2) + cp2) % 2
                for cc in range(2):
                    c = cp2 * 2 + cc
                    for hp in range(2):
                        p.tr(bks(b, (slice(0, 64), slice(cc * 256 + hp * 128, cc * 256 + hp * 128 + 128))),
                             src[hp][:, c * 64:(c + 1) * 64], ident, inc=(cc == 1 and hp == 1))
                eng = "act" if (cp2 % 2 == 0) else "dve"
                p.cp(eng, dst[:, cp2 * 2:cp2 * 2 + 2, :].rr("p a b -> p (a b)"), bks(b, (slice(0, 64), slice(0, 512))))
        for c in range(NCH):
            G = s["GTs"][c % 2]
            csl = slice(c * 64, (c + 1) * 64)
            for h in range(4):
                hp, sl = h // 2, slice((h % 2) * 64, (h % 2) * 64 + 64)
                gb = 4 + h // 2
                o = (h % 2) * 256
                p.mm(bks(gb, (slice(0, 64), slice(o, o + 128))), bG[hp][sl, csl], arG[hp][sl, c, :], inc=False)
                p.mm(bks(gb, (slice(0, 64), slice(o + 128, o + 256))), kG[hp][sl, csl], arG[hp][sl, c, :], inc=(h % 2 == 1))
            if scalar_mode:
                for h in range(4):
                    p.mm(bks(6, (slice(0, 64), slice(h * 64, h * 64 + 64))), cs[0:4, C_SEL + h * 64:C_SEL + h * 64 + 64],
                         gam[0:4, csl], inc=(h == 3))
                for h in range(4):
                    p.stt("dve", s["DT"][:, h * 64:h * 64 + 64], bks(6, (slice(0, 64), slice(h * 64, h * 64 + 64))),
                          s["gcol"][:, c * 4 + h:c * 4 + h + 1], cs[0:64, C_MNI:C_MNI + 64], ALU.subtract, ALU.add)
                p.act(s["DT"][:], s["DT"][:], AF.Exp)
                dt4 = View(s["DT"][:].ap.rearrange("p (h t) -> p h t", h=4).unsqueeze(2).broadcast_to([64, 4, 4, 64]), s["DT"][:].keys)
                p.tt("dve", s["DTm"][:].rr("p (h b t) -> p h b t", h=4, b=4), cs[0:64, C_MT4:C_MT4 + 1024].rr("p (h b t) -> p h b t", h=4, b=4),
                     dt4, ALU.mult)
                mk = lambda half: s["DTm"][:, half * 512:half * 512 + 512]
            else:
                mk = lambda half: cs[0:64, C_MT4 + half * 512: C_MT4 + half * 512 + 512]
            for half in range(2):
                p.tt("dve", G[:, half * 2:half * 2 + 2, :].rr("p a b -> p (a b)"), bks(4 + half, (slice(0, 64), slice(0, 512))),
                     mk(half), ALU.mult)
            M0 = s["M"][0]
            p.cp("act", M0[:, 1, :].rr("p (h t) -> p h t", h=4), G[:, :, 0:64])
            for h in range(4):
                p.tr(bks(6, (slice(0, 64), slice(h * 64, h * 64 + 64))), G[:, h, 0:64], cs[0:64, C_ID:C_ID + 64], inc=(h == 3))
            p.cp("dve", M0[:, 0, :], bks(6, (slice(0, 64), slice(0, 256))))
            Tt = s["Tt"]
            p.tt("dve", Tt[:, c, :], M0[:, 1, :], cs[0:64, C_I64:C_I64 + 256], ALU.add)
            cur = 0
            for lvl in range(5):
                Mc = s["M"][cur]
                Mn = s["M"][1 - cur]
                lastl = (lvl == 4)
                for h in range(4):
                    hs = slice(h * 64, h * 64 + 64)
                    p.mm(bks(7, (slice(0, 64), hs)), Mc[:, 1, hs], Mc[:, 0, hs], inc=(lastl and h == 3))
                if not lastl:
                    for h in range(4):
                        hs = slice(h * 64, h * 64 + 64)
                        p.mm(bks(7, (slice(0, 64), slice(256 + h * 64, 256 + h * 64 + 64))), Mc[:, 0, hs], Mc[:, 1, hs], inc=(h == 3))
                    p.cp("act", Mn[:].rr("p a b -> p (a b)"), bks(7, (slice(0, 64), slice(0, 512))))
                else:
                    p.cp("act", Mn[:, 0, :], bks(7, (slice(0, 64), slice(0, 256))))
                for h in range(4):
                    hs = slice(h * 64, h * 64 + 64)
                    p.mm(bks(6, (slice(0, 64), slice(256 + h * 64, 256 + h * 64 + 64))), Mn[:, 0, hs], Tt[:, c, hs], inc=(h == 3))
                p.tt("dve", Tt[:, c, :], Tt[:, c, :], bks(6, (slice(0, 64), slice(256, 512))), ALU.add)
                cur = 1 - cur
            Xs, Us = s["Xs"], s["Us"]
            for h in range(4):
                hp, sl = h // 2, slice((h % 2) * 64, (h % 2) * 64 + 64)
                hs = slice(h * 64, h * 64 + 64)
                o = bks(0, (slice(0, 64), hs))
                p.mm(o, arT[hp][sl, c, 0:64], H[hp][sl, :], start=True, stop=False, inc=False)
                p.mm(o, G[:, h, 128:192], s["tmV"][:, c, hs], start=False, stop=True, inc=(h == 3))
            p.cp("act", Xs[:], bks(0, (slice(0, 64), slice(0, 256))))
            for h in range(4):
                hs = slice(h * 64, h * 64 + 64)
                p.mm(bks(0, (slice(0, 64), slice(256 + h * 64, 256 + h * 64 + 64))), Tt[:, c, hs], Xs[:, hs], inc=(h == 3))
            p.cp("dve", Us[:], bks(0, (slice(0, 64), slice(256, 512))))
            for h in range(4):
                hp, sl = h // 2, slice((h % 2) * 64, (h % 2) * 64 + 64)
                hs = slice(h * 64, h * 64 + 64)
                yb = 1 if hp == 0 else 3
                o = bks(yb, (sl, csl))
                p.mm(o, H[hp][sl, :], arT[hp][sl, c, 64:128], start=True, stop=False, inc=False)
                p.mm(o, Us[:, hs], G[:, h, 64:128], start=False, stop=False, inc=False)
                p.mm(o, s["tmV"][:, c, hs], G[:, h, 192:256], start=False, stop=True, inc=(h % 2 == 1))
            if scalar_mode:
                for hp in range(2):
                    p.act(s["Hs"][hp][:], H[hp][:], AF.Copy, scale=pc(hp, c))
                Hsrc = s["Hs"]
            else:
                Hsrc = H
            for h in range(4):
                hp, sl = h // 2, slice((h % 2) * 64, (h % 2) * 64 + 64)
                hs = slice(h * 64, h * 64 + 64)
                o = bks(2, (sl, slice(hp * 64, hp * 64 + 64)))
                p.mm(o, cs[sl, C_ID + (h % 2) * 64: C_ID + (h % 2) * 64 + 64], Hsrc[hp][sl, :], start=True, stop=False, inc=False)
                p.mm(o, s["tmB"][:, c, hs], Us[:, hs], start=False, stop=False, inc=False)
                p.mm(o, s["tmK"][:, c, hs], s["tmV"][:, c, hs], start=False, stop=True, inc=(h % 2 == 1))
            for hp in range(2):
                if scalar_mode:
                    p.cp("act", H[hp][:], bks(2, (slice(0, 128), slice(hp * 64, hp * 64 + 64))))
                else:
                    p.act(H[hp][:], bks(2, (slice(0, 128), slice(hp * 64, hp * 64 + 64))), AF.Copy, scale=pc(hp, c))
        for hp in range(2):
            yb = 1 if hp == 0 else 3
            p.cp("act" if hp == 0 else "dve", yout[hp][:], bk(yb))

    def phase_mix(self, l):
        p = self.p
        cs = self.cs
        pp = self.ppt
        with contextlib.ExitStack() as st:
            banks = self.ps(st, 8)
            bk = lambda i: dv(banks[i][:, :], f"pb{i}")
            bks = lambda i, sl: dv(banks[i][sl], f"pb{i}")
            self.bm = self.sb(st, [128, 2 * 8 * 128], F32, "biasm")
            p.dma("sp", self.bm[:], dv(self.bband.ap(), "bb_d"))
            for kind in range(2):
                for h in range(8):
                    o = self.bm[:, (kind * 8 + h) * 128:(kind * 8 + h + 1) * 128]
                    p.tt("dve", o, o, self.cs[:, C_BM + kind * 128: C_BM + (kind + 1) * 128], ALU.add)
            s = self.dplr_setup(st)
            W = [self.sb(st, [128, TT + 3], F32, "W") for _ in range(34)]
            arT = [self.sb(st, [128, NCH, 128], F32, "arT") for _ in range(2)]
            arG = [self.sb(st, [128, NCH, 128], F32, "arG") for _ in range(2)]
            Hst = [self.sb(st, [128, 64], F32, "H") for _ in range(2)]
            sm = [self.sb(st, [4, TT], F32, "sm") for _ in range(11)]
            yb16 = [self.sb(st, [128, TT], BF16, "yb") for _ in range(2)]
            wup = self.sb(st, [128, 256], F32, "wup")
            aup = self.sb(st, [128, 256], F32, "aup")
            gup = self.sb(st, [128, 256], F32, "gup")
            p.dma("sp", wup[0:64, :], dv(self.w_up.ap()[l], "wup_d"))
            p.dma("sp", aup[64:128, :], dv(self.a_up.ap()[l], "aup_d"))
            p.dma("sp", gup[:, :], dv(self.g_up.ap()[l], "gup_d"))
            b1 = cs[:, C_B1:C_B1 + 128]
            b64 = cs[:, C_B64:C_B64 + 128]
            rst = cs[:, C_RST:C_RST + TT]
            zd = self.z_d.ap()
            col = lambda c: pp[:, c:c + 1]

            def load_halo(tile, row0, nrows, t0, j, halo, first, gkeys, prow=0):
                rows = slice(prow, prow + nrows)
                if first:
                    if halo > 0:
                        p.memset("pool", tile[rows, 0:halo], 0.0)
                    p.dma("sp", tile[rows, halo:halo + TT], dv(zd[row0:row0 + nrows, t0:t0 + TT], *[(("z",) + (g, j)) for g in gkeys]))
                else:
                    p.dma("sp", tile[rows, 0:halo + TT], dv(zd[row0:row0 + nrows, t0 - halo:t0 + TT],
                                                           *([(("z",) + (g, j)) for g in gkeys] + [(("z",) + (g, j - 1)) for g in gkeys])))

            def zkey(row0):
                g = row0 // 128
                return [g - (g % 2), g - (g % 2) + 1]

            def l2rn(x, tmp, tmp2, bank, eps, blk):
                p.act(tmp[:, 0:TT], x, AF.Square)
                p.mm(bk(bank), blk, tmp[:, 0:TT])
                p.act(tmp2[:, 0:TT], bk(bank), AF.Sqrt, bias=eps)
                p.recip(tmp2[:, 0:TT], tmp2[:, 0:TT])

            for sq in range(self.mix_nseq):
                for hp in range(2):
                    p.memset("pool", Hst[hp][:], 0.0)
                for jt in range(self.mix_tiles if "rwkv" in self.mix_parts else 0):
                    j = sq * TPS + jt
                    t0 = j * TT
                    first = (jt == 0)
                    Z = W[0:8]
                    XS = W[8:16]
                    for role in range(8):
                        load_halo(Z[role], role * 128, 128, t0, j, 1, first, zkey(role * 128))
                        eng = "dve" if role % 2 == 0 else "pool"
                        p.tt(eng, W[16][:, 0:TT], Z[role][:, 0:TT], Z[role][:, 1:TT + 1], ALU.subtract)
                        p.stt(eng, XS[role][:, 0:TT], W[16][:, 0:TT], col(P_MU + role), Z[role][:, 1:TT + 1], ALU.mult, ALU.add)
                    xr, xk, xv, xdd, xdg = XS[0:2], XS[2:4], XS[4:6], XS[6], XS[7]
                    p.act(xdd[0:64, 0:TT], xdd[0:64, 0:TT], AF.Tanh)
                    p.act(xdg[:, 0:TT], xdg[:, 0:TT], AF.Sigmoid)
                    lw, iclr, gate, kk = W[0:2], W[2:4], W[4:6], W[6:8]
                    cum, Pin, Pex, Pinv = W[17:19], W[19:21], W[21:23], W[23:25]
                    bT, kT, bon = W[25:27], W[27:29], W[29:31]
                    yo = W[31:33]
                    tmp = W[16]
                    tmp2 = W[33]
                    for hp in range(2):
                        hsl = slice(hp * 128, hp * 128 + 128)
                        p.mm(bk(0), wup[0:64, hsl], xdd[0:64, 0:TT])
                        p.act(lw[hp][:, 0:TT], bk(0), AF.Sigmoid, bias=col(P_W0 + hp))
                        p.ts("dve", lw[hp][:, 0:TT], lw[hp][:, 0:TT], DECAY_C, ALU.mult)
                        p.mm(bk(1), aup[64:128, hsl], xdd[64:128, 0:TT])
                        p.act(iclr[hp][:, 0:TT], bk(1), AF.Sigmoid, bias=col(P_A0 + hp))
                        p.mm(bk(0), gup[:, hsl], xdg[:, 0:TT])
                        p.cp("act", gate[hp][:, 0:TT], bk(0))
                        p.ts("dve", kk[hp][:, 0:TT], xk[hp][:, 0:TT], col(P_KK + hp), ALU.mult)
                        l2rn(kk[hp][:, 0:TT], tmp, tmp2, 1, 1e-6, b1)
                        p.tt("dve", kk[hp][:, 0:TT], kk[hp][:, 0:TT], tmp2[:, 0:TT], ALU.mult)
                        p.ts("pool", tmp[:, 0:TT], iclr[hp][:, 0:TT], -1.0, ALU.add, col(P_KA + hp), ALU.mult)
                        p.stt("pool", xk[hp][:, 0:TT], tmp[:, 0:TT], 1.0, xk[hp][:, 0:TT], ALU.add, ALU.mult)
                        p.scan(cum[hp][:, 0:TT], rst, lw[hp][:, 0:TT])
                        p.act(Pin[hp][:, 0:TT], cum[hp][:, 0:TT], AF.Exp)
                        p.act(Pinv[hp][:, 0:TT], cum[hp][:, 0:TT], AF.Exp, scale=-1.0)
                        p.tt("dve", tmp[:, 0:TT], cum[hp][:, 0:TT], lw[hp][:, 0:TT], ALU.subtract)
                        p.act(Pex[hp][:, 0:TT], tmp[:, 0:TT], AF.Exp)
                        a3 = arT[hp][:]
                        v3 = lambda t: t[:, 0:TT].rr("p (c t) -> p c t", t=64)
                        p.stt("dve", a3[:, :, 0:64], v3(kk[hp]), -1.0, v3(Pex[hp]), ALU.mult, ALU.mult)
                        p.tt("pool", a3[:, :, 64:128], v3(xr[hp]), v3(Pin[hp]), ALU.mult)
                        p.tt("dve", tmp[:, 0:TT], kk[hp][:, 0:TT], iclr[hp][:, 0:TT], ALU.mult)
                        p.tt("dve", bT[hp][:, 0:TT], tmp[:, 0:TT], Pinv[hp][:, 0:TT], ALU.mult)
                        p.tt("pool", kT[hp][:, 0:TT], xk[hp][:, 0:TT], Pinv[hp][:, 0:TT], ALU.mult)
                        p.stt("dve", tmp[:, 0:TT], xr[hp][:, 0:TT], col(P_RK + hp), xk[hp][:, 0:TT], ALU.mult, ALU.mult)
                        p.mm(bk(1), b1, tmp[:, 0:TT])
                        p.tt("dve", bon[hp][:, 0:TT], bk(1), xv[hp][:, 0:TT], ALU.mult)
                    pc = lambda hp, c: Pin[hp][:, c * 64 + 63: c * 64 + 64]
                    self.dplr_tile(s, banks, arT, [t[:, 0:TT] for t in bT], [t[:, 0:TT] for t in kT],
                                   [t[:, 0:TT] for t in xv], pc, Hst, [t[:, 0:TT] for t in yo])
                    for hp in range(2):
                        y = yo[hp][:, 0:TT]
                        p.mm(bk(0), b64, y)
                        p.tt("dve", y, y, bk(0), ALU.subtract)
                        l2rn(y, tmp, tmp2, 1, 64e-5, b64)
                        p.tt("dve", y, y, tmp2[:, 0:TT], ALU.mult)
                        p.ts("dve", y, y, col(P_LXW + hp), ALU.mult, col(P_LXB + hp), ALU.add)
                        p.tt("pool", y, y, bon[hp][:, 0:TT], ALU.add)
                        p.tt("dve", yb16[hp][:], y, gate[hp][:, 0:TT], ALU.mult)
                        p.dma("pool", dv(self.y_d.ap()[hp * 128:(hp + 1) * 128, t0:t0 + TT], ("y", hp, j)), yb16[hp][:])
                for hp in range(2):
                    p.memset("pool", Hst[hp][:], 0.0)
                for jt in range(self.mix_tiles if "gdn" in self.mix_parts else 0):
                    j = sq * TPS + jt
                    t0 = j * TT
                    first = (jt == 0)
                    Z = W[0:6]
                    CV = W[6:12]
                    gt = W[12:14]
                    for i in range(6):
                        r0 = 1024 + i * 128
                        load_halo(Z[i], r0, 128, t0, j, 3, first, zkey(r0))
                        eng = "dve" if i % 2 == 0 else "pool"
                        cw = lambda k: col(P_CONV + i * 4 + k)
                        o = CV[i][:, 0:TT]
                        p.ts(eng, o, Z[i][:, 0:TT], cw(0), ALU.mult)
                        for k in range(1, 4):
                            p.stt(eng, o, Z[i][:, k:k + TT], cw(k), o, ALU.mult, ALU.add)
                        p.act(o, o, AF.Silu)
                    for hp in range(2):
                        r0 = 1792 + hp * 128
                        load_halo(gt[hp], r0, 128, t0, j, 0, True, zkey(r0))
                        p.act(gt[hp][:, 0:TT], gt[hp][:, 0:TT], AF.Silu)
                    bl, al = sm[0], sm[1]
                    p.dma("sp", bl[:], dv(zd[2048:2052, t0:t0 + TT], ("z", 16, j), ("z", 17, j)))
                    p.dma("sp", al[:], dv(zd[2052:2056, t0:t0 + TT], ("z", 16, j), ("z", 17, j)))
                    beta, xx, ax, ee, gg, gam, E1, nb, BE1n, Ff, BF = sm[0:11]
                    p.act(beta[:], bl[:], AF.Sigmoid)
                    p.ts("dve", xx[:], al[:], pp[0:4, P_DTB:P_DTB + 1], ALU.add)
                    p.act(ax[:], xx[:], AF.Abs)
                    p.act(ee[:], ax[:], AF.Exp, scale=-1.0)
                    p.act(ee[:], ee[:], AF.Ln, bias=1.0)
                    p.stt("dve", gg[:], xx[:], 0.0, ee[:], ALU.max, ALU.add)
                    p.ts("dve", gg[:], gg[:], self.der[0:4, 1:2], ALU.mult)
                    p.scan(gam[:], cs[0:4, C_RST:C_RST + TT], gg[:])
                    p.act(E1[:], gam[:], AF.Exp)
                    p.ts("dve", nb[:], beta[:], -1.0, ALU.mult)
                    p.tt("dve", BE1n[:], nb[:], E1[:], ALU.mult)
                    g3 = gam[:].rr("p (c t) -> p c t", t=64)
                    gend = View(g3.ap[:, :, 63:64].broadcast_to([4, NCH, 64]), g3.keys)
                    p.tt("dve", Ff[:].rr("p (c t) -> p c t", t=64), gend, g3, ALU.subtract)
                    p.act(Ff[:], Ff[:], AF.Exp)
                    p.tt("dve", BF[:], beta[:], Ff[:], ALU.mult)
                    cq, ck, cv = CV[0:2], CV[2:4], CV[4:6]
                    bT, kT, yo = W[14:16], W[16:18], W[18:20]
                    tmp, tmp2 = W[20], W[21]
                    pct = W[22]
                    kG = W[23:25]
                    for hp in range(2):
                        l2rn(cq[hp][:, 0:TT], tmp, tmp2, 0, 1e-6, b1)
                        p.stt("dve", cq[hp][:, 0:TT], cq[hp][:, 0:TT], 0.125, tmp2[:, 0:TT], ALU.mult, ALU.mult)
                        l2rn(ck[hp][:, 0:TT], tmp, tmp2, 1, 1e-6, b1)
                        p.tt("dve", ck[hp][:, 0:TT], ck[hp][:, 0:TT], tmp2[:, 0:TT], ALU.mult)
                        a3 = arT[hp][:]
                        g3a = arG[hp][:]
                        v3 = lambda t: t[:, 0:TT].rr("p (c t) -> p c t", t=64)
                        pv3 = lambda b: bk(b).rr("p (c t) -> p c t", t=64)

                        def bcast(bank, fac):
                            for hh in range(2):
                                h = hp * 2 + hh
                                p.mm(bks(bank, (slice(hh * 64, hh * 64 + 64), slice(0, TT))), cs[0:4, C_SEL + h * 64: C_SEL + h * 64 + 64],
                                     fac[:], inc=(hh == 1))
                        bcast(0, nb)
                        p.tt("dve", g3a[:, :, 0:64], v3(ck[hp]), pv3(0), ALU.mult)
                        p.cp("pool", g3a[:, :, 64:128], v3(cq[hp]))
                        bcast(1, BE1n)
                        p.tt("dve", a3[:, :, 0:64], v3(ck[hp]), pv3(1), ALU.mult)
                        bcast(0, E1)
                        p.tt("dve", a3[:, :, 64:128], v3(cq[hp]), pv3(0), ALU.mult)
                        p.cp("act", pct[:, hp * 8:hp * 8 + 8], bk(0).rr("p (c t) -> p c t", t=64)[:, :, 63])
                        bcast(1, beta)
                        p.tt("dve", kG[hp][:, 0:TT], ck[hp][:, 0:TT], bk(1), ALU.mult)
                        bcast(0, Ff)
                        p.tt("dve", bT[hp][:, 0:TT], ck[hp][:, 0:TT], bk(0), ALU.mult)
                        bcast(1, BF)
                        p.tt("dve", kT[hp][:, 0:TT], ck[hp][:, 0:TT], bk(1), ALU.mult)
                    pc = lambda hp, c: pct[:, hp * 8 + c: hp * 8 + c + 1]
                    self.dplr_tile(s, banks, arT, [t[:, 0:TT] for t in bT], [t[:, 0:TT] for t in kT],
                                   [t[:, 0:TT] for t in cv], pc, Hst, [t[:, 0:TT] for t in yo],
                                   arG=arG, bG=[t[:, 0:TT] for t in ck], kG=[t[:, 0:TT] for t in kG], gam=gam)
                    for hp in range(2):
                        y = yo[hp][:, 0:TT]
                        l2rn(y, tmp, tmp2, 0, 1e-6, b64)
                        p.stt("dve", y, y, col(P_GNW), tmp2[:, 0:TT], ALU.mult, ALU.mult)
                        p.tt("dve", yb16[hp][:], y, gt[hp][:, 0:TT], ALU.mult)
                        p.dma("pool", dv(self.y_d.ap()[256 + hp * 128:256 + (hp + 1) * 128, t0:t0 + TT], ("y", 2 + hp, j)), yb16[hp][:])
                if "attn" in self.mix_parts:
                    self.attn_seq(sq, W, banks, yb16, st)

    def attn_seq(self, sq, W, banks, yb16, st):
        p = self.p
        cs = self.cs
        pp = self.ppt
        bk = lambda i: dv(banks[i][:, :], f"pb{i}")
        bks = lambda i, sl: dv(banks[i][sl], f"pb{i}")
        zd = self.z_d.ap()
        ident = cs[:, C_ID:C_ID + 128]
        b64 = cs[:, C_B64:C_B64 + 128]
        if not hasattr(self, "_attn_tiles"):
            v1 = self.sb(st, [128, 5, 2, 65], F32, "v1")
            kt = self.sb(st, [128, 128 + TT], F32, "kt")
            osb = self.sb(st, [128, 512], F32, "osb")
            den = self.sb(st, [128, 8], F32, "den")
            ya = [self.sb(st, [128, TT], BF16, "ya") for _ in range(4)]
            self._attn_tiles = (v1, kt, osb, den, ya)
        v1, kt, osb, den, ya = self._attn_tiles
        for jt in range(self.mix_tiles):
            j = sq * TPS + jt
            t0 = j * TT
            first = (jt == 0)
            Q = W[0:4]
            tmp, tmp2 = W[4], W[5]
            E = W[6:10]
            p.memset("pool", v1[:, :, :, 64:65], 1.0)
            for t in range(4):
                for g in range(2):
                    h = g * 4 + t
                    r0 = 2056 + h * 64
                    gk = r0 // 128
                    gk2 = (r0 + 63) // 128
                    keys = set()
                    for gg in (gk, gk2):
                        keys.add(("z", gg - (gg % 2), j))
                        keys.add(("z", gg - (gg % 2) + 1, j))
                    p.dma("sp", Q[t][g * 64:g * 64 + 64, 0:TT], dv(zd[r0:r0 + 64, t0:t0 + TT], *keys))
            kkeys = [("z", 20, j), ("z", 21, j)]
            if first:
                p.memset("pool", kt[:, 0:128], 0.0)
                p.dma("sp", kt[:, 128:128 + TT], dv(zd[2568:2696, t0:t0 + TT], *kkeys))
            else:
                p.dma("sp", kt[:, 0:128 + TT], dv(zd[2568:2696, t0 - 128:t0 + TT], *(kkeys + [("z", 20, j - 1), ("z", 21, j - 1)])))
            vt = self.vtm_d.ap()
            if not first:
                p.dma("sp", v1[:, 0, :, 0:64], dv(vt[t0 - 128:t0, :].rearrange("p (g d) -> p g d", g=2), ("vtm", j - 1)))
            for q in range(4):
                p.dma("sp", v1[:, 1 + q, :, 0:64], dv(vt[t0 + q * 128:t0 + (q + 1) * 128, :].rearrange("p (g d) -> p g d", g=2), ("vtm", j)))
            for t in range(4):
                q = Q[t][:, 0:TT]
                p.act(tmp[:, 0:TT], q, AF.Square)
                p.mm(bk(0), b64, tmp[:, 0:TT])
                p.act(tmp2[:, 0:TT], bk(0), AF.Sqrt, bias=1e-6)
                p.recip(tmp2[:, 0:TT], tmp2[:, 0:TT])
                p.stt("dve", q, q, self.der[:, 0:1], tmp2[:, 0:TT], ALU.mult, ALU.mult)
            for part in range(2):
                if part == 0:
                    if first:
                        continue
                    ks = slice(0, 128)
                else:
                    ks = slice(128, 128 + TT)
                n = ks.stop - ks.start
                k = kt[:, ks]
                p.act(tmp[:, 0:n], k, AF.Square)
                p.mm(bks(1, (slice(0, 128), slice(0, n))), b64, tmp[:, 0:n])
                p.act(tmp2[:, 0:n], bks(1, (slice(0, 128), slice(0, n))), AF.Sqrt, bias=1e-6)
                p.recip(tmp2[:, 0:n], tmp2[:, 0:n])
                p.stt("dve", k, k, pp[:, P_KNW:P_KNW + 1], tmp2[:, 0:n], ALU.mult, ALU.mult)
            for n in range(4 if CUT > 1 else 0):
                qs = slice(n * 128, (n + 1) * 128)
                noprev = first and n == 0
                for g in range(2):
                    gs = slice(g * 64, g * 64 + 64)
                    for kind in range(2):
                        if kind == 0 and noprev:
                            continue
                        kcols = slice(n * 128 + kind * 128, n * 128 + kind * 128 + 128)
                        b = 2 + kind
                        for t in range(4):
                            p.mm(bks(b, (slice(0, 128), slice(t * 128, t * 128 + 128))), kt[gs, kcols], Q[t][gs, qs], inc=(t == 3))
                        Ek = E[kind * 2 + (g % 2)]
                        bo = (kind * 8 + g * 4) * 128
                        p.tt("dve", Ek[:, 0:TT], bk(b), self.bm[:, bo:bo + 512], ALU.add)
                        p.act(Ek[:, 0:TT], Ek[:, 0:TT], AF.Exp)
                    ob = 4 + g
                    if CUT <= 2:
                        continue
                    for t in range(4):
                        o = bks(ob, (slice(0, 128), slice(t * 65, t * 65 + 65)))
                        if not noprev:
                            p.mm(o, E[0 + g][:, t * 128:t * 128 + 128], v1[:, n, g, :], start=True, stop=False, inc=False)
                        p.mm(o, E[2 + g][:, t * 128:t * 128 + 128], v1[:, n + 1, g, :], start=noprev, stop=True, inc=(t == 3))
                    if CUT <= 3:
                        continue
                    o3 = bks(ob, (slice(0, 128), slice(0, 260))).rr("p (t e) -> p t e", e=65)
                    p.tt("dve", den[:, g * 4:g * 4 + 4], o3[:, :, 64], self.der[:, 2 + g * 4: 2 + g * 4 + 4], ALU.add)
                    p.recip(den[:, g * 4:g * 4 + 4], den[:, g * 4:g * 4 + 4])
                    for t in range(4):
                        h = g * 4 + t
                        if t % 2 == 0:
                            p.ts("dve", osb[:, h * 64:h * 64 + 64], o3[:, t, 0:64], den[:, h:h + 1], ALU.mult)
                        else:
                            p.act(osb[:, h * 64:h * 64 + 64], o3[:, t, 0:64], AF.Copy, scale=den[:, h:h + 1])
                TRV = int(os.environ.get("TRV", "0"))
                for hp4 in range(4 if CUT > 4 else 0):
                    tb = (6 if TRV != 1 else 0) + hp4 // 2
                    src_t = osb if TRV != 2 else Q[0]
                    p.tr(bks(tb, (slice(0, 128), slice((hp4 % 2) * 128, (hp4 % 2) * 128 + 128))),
                         src_t[:, hp4 * 128:(hp4 + 1) * 128], ident, inc=(hp4 % 2 == 1) or TRV == 3)
                for hh in range(2 if (CUT > 4 and CUT != 7) else 0):
                    for q2 in range(2):
                        hp4 = hh * 2 + q2
                        src = bks(6 + hh, (slice(0, 128), slice(q2 * 128, q2 * 128 + 128)))
                        if q2 == 0:
                            p.cp("act", ya[hp4][:, qs], src)
                        else:
                            p.cp("dve", ya[hp4][:, qs], src)
            for hp4 in range(4 if CUT not in (6, 7) else 0):
                p.dma("pool", dv(self.y_d.ap()[512 + hp4 * 128:512 + (hp4 + 1) * 128, t0:t0 + TT], ("y", 4 + hp4, j)), ya[hp4][:])

    def phase_outproj(self, l, xsrc, xdst):
        p = self.p
        if hasattr(self, "_attn_tiles"):
            del self._attn_tiles
        with contextlib.ExitStack() as st:
            wb = self.sb(st, [128, 8, D], BF16, "woutb")
            for c in range(8):
                p.dma("pool", wb[:, c, :], dv(self.w_out.ap()[l, c * 128:(c + 1) * 128, :], "w_out_d"))
            xts = [self.sb(st, [128, 8, TT], F32, "xt") for _ in range(2)]
            yts = [self.sb(st, [128, 8, TT], BF16, "yt") for _ in range(2)]
            banks = self.ps(st, 8)
            xs_v = xsrc.ap().rearrange("(c p) t -> p c t", p=128)
            xd_v = xdst.ap().rearrange("(c p) t -> p c t", p=128)
            yv = self.y_d.ap().rearrange("(c p) t -> p c t", p=128)
            for j in range(NTILE):
                t0 = j * TT
                xt, yt = xts[j % 2], yts[j % 2]
                for hh in range(2):
                    p.dma("sp", xt[:, hh * 4:(hh + 1) * 4, :], dv(xs_v[:, hh * 4:(hh + 1) * 4, t0:t0 + TT], ("x", id(xsrc), j)))
                p.dma("sp", yt[:], dv(yv[:, :, t0:t0 + TT], *[("y", c, j) for c in range(8)]))
                for m in range(8):
                    pv = dv(banks[m][:, :], f"pb{m}")
                    for c in range(8):
                        p.mm(pv, wb[:, c, m * 128:(m + 1) * 128], yt[:, c, :], start=(c == 0), stop=(c == 7), inc=(c == 7))
                    p.tt("dve", xt[:, m, :], xt[:, m, :], pv, ALU.add)
                for hh in range(2):
                    p.dma("pool", dv(xd_v[:, hh * 4:(hh + 1) * 4, t0:t0 + TT], ("x", id(xdst), j)), xt[:, hh * 4:(hh + 1) * 4, :])

    def phase_ffn(self, l, xsrc, xdst):
        p = self.p
        with contextlib.ExitStack() as st:
            w1 = self.sb(st, [128, 8, DFF], BF16, "w1b")
            w2 = self.sb(st, [128, 32, D], BF16, "w2b")
            for c in range(8):
                p.dma("pool", w1[:, c, :], dv(self.w_ff1.ap()[l, c * 128:(c + 1) * 128, :], "w1_d"))
            w2v = self.w_ff2.ap()[l].rearrange("(c p) n -> p c n", p=128)
            for c4 in range(8):
                p.dma("pool", w2[:, c4 * 4:(c4 + 1) * 4, :], dv(w2v[:, c4 * 4:(c4 + 1) * 4, :], "w2_d"))
            xt = self.sb(st, [128, 8, TT], F32, "xt")
            ht = self.sb(st, [128, 8, TT], BF16, "ht")
            h1 = self.sb(st, [128, 32, TT], BF16, "h1")
            sq2 = [self.sb(st, [128, TT], F32, "sq") for _ in range(2)]
            rstd = self.sb(st, [128, TT], F32, "rstd")
            banks = self.ps(st, 8)
            xs_v = xsrc.ap().rearrange("(c p) t -> p c t", p=128)
            xd_v = xdst.ap().rearrange("(c p) t -> p c t", p=128)
            for j in range(NTILE):
                t0 = j * TT
                for hh in range(2):
                    p.dma("sp", xt[:, hh * 4:(hh + 1) * 4, :], dv(xs_v[:, hh * 4:(hh + 1) * 4, t0:t0 + TT], ("x", id(xsrc), j)))
                self.rms_tile(xt, ht, P_LN2, sq2, rstd, banks[7], "pb7")
                for f in range(32):
                    b = f % 6
                    pv = dv(banks[b][:, :], f"pb{b}")
                    for c in range(8):
                        p.mm(pv, w1[:, c, f * 128:(f + 1) * 128], ht[:, c, :], start=(c == 0), stop=(c == 7), inc=(c == 7))
                    s = sq2[f % 2]
                    p.act(s[:], pv, AF.Relu)
                    p.tt("dve" if f % 2 == 0 else "pool", h1[:, f, :], s[:], s[:], ALU.mult)
                for m in range(8):
                    b = m % 6
                    pv = dv(banks[b][:, :], f"pb{b}")
                    for f in range(32):
                        p.mm(pv, w2[:, f, m * 128:(m + 1) * 128], h1[:, f, :], start=(f == 0), stop=(f == 31), inc=(f == 31))
                    p.tt("dve", xt[:, m, :], xt[:, m, :], pv, ALU.add)
                for hh in range(2):
                    p.dma("pool", dv(xd_v[:, hh * 4:(hh + 1) * 4, t0:t0 + TT], ("x", id(xdst), j)), xt[:, hh * 4:(hh + 1) * 4, :])


def _t5_bucket_np(dist):
    max_exact = 16
    nf = np.maximum(dist, max_exact).astype(np.float32)
    large = max_exact + (np.log(nf / max_exact) / np.float32(np.log(128 / max_exact)) * (32 - max_exact)).astype(np.int32)
    large = np.minimum(large, 31)
    return np.where(dist < max_exact, dist, large)


def _consts():
    c = np.zeros((128, NCST), np.float32)
    c[:, C_ID:C_ID + 128] = np.eye(128, dtype=np.float32)
    blk = np.kron(np.eye(2, dtype=np.float32), np.ones((64, 64), np.float32))
    c[:, C_B1:C_B1 + 128] = blk
    c[:, C_B64:C_B64 + 128] = blk / 64.0
    c[:, C_OD:C_OD + 128] = 1.0 / 1024.0
    rst = np.ones((128, TT), np.float32)
    rst[:, ::64] = 0.0
    c[:, C_RST:C_RST + TT] = rst
    s_ = np.arange(64)[:, None]
    t_ = np.arange(64)[None, :]
    strict = (s_ < t_).astype(np.float32)
    incl = (s_ <= t_).astype(np.float32)
    mt = np.concatenate([strict, incl, strict, incl], 1)
    c[0:64, C_MT4:C_MT4 + 1024] = np.tile(mt, (1, 4))
    msl = (t_ < s_).astype(np.float32)
    c[0:64, C_MSL4:C_MSL4 + 256] = np.tile(msl, (1, 4))
    c[0:64, C_I64:C_I64 + 256] = np.tile(np.eye(64, dtype=np.float32), (1, 4))
    for h in range(4):
        c[h, C_SEL + h * 64:C_SEL + (h + 1) * 64] = 1.0
    c[0:64, C_MNI:C_MNI + 64] = np.where(s_ <= t_, 0.0, NEG)
    jj = np.arange(128)[:, None]
    ii = np.arange(128)[None, :]
    c[:, C_BM:C_BM + 128] = np.where(jj > ii, 0.0, NEG)
    c[:, C_BM + 128:C_BM + 256] = np.where(jj <= ii, 0.0, NEG)
    return c


def _bias_band(rel_bias):
    i = np.arange(128)[None, :]
    j = np.arange(128)[:, None]
    out = np.zeros((128, 2, 8, 128), np.float32)
    d_prev = np.maximum(i + 128 - j, 0)
    d_cur = np.maximum(i - j, 0)
    for kind, dd in enumerate((d_prev, d_cur)):
        b = _t5_bucket_np(dd)
        out[:, kind, :, :] = np.transpose(rel_bias[b], (0, 2, 1))
    return out.reshape(128, 2 * 8 * 128)


def _pack_params(inp, l):
    pp = np.zeros((128, NPP), np.float32)
    f = lambda a, n: np.asarray(a, np.float32).reshape(n, 128).T
    pp[:, P_LN1:P_LN1 + 8] = f(inp["ln1_w"][l], 8)
    pp[:, P_LN2:P_LN2 + 8] = f(inp["ln2_w"][l], 8)
    pp[:, P_MU:P_MU + 8] = f(inp["rwkv_mu"][l], 8)
    pp[:, P_W0:P_W0 + 2] = f(inp["rwkv_w0"][l], 2)
    pp[:, P_A0:P_A0 + 2] = f(inp["rwkv_a0"][l], 2)
    pp[:, P_KK:P_KK + 2] = f(inp["rwkv_k_k"][l], 2)
    pp[:, P_KA:P_KA + 2] = f(inp["rwkv_k_a"][l], 2)
    pp[:, P_RK:P_RK + 2] = f(inp["rwkv_r_k"][l].reshape(256), 2)
    pp[:, P_LXW:P_LXW + 2] = f(inp["rwkv_lnx_w"][l], 2)
    pp[:, P_LXB:P_LXB + 2] = f(inp["rwkv_lnx_b"][l], 2)
    cw = np.asarray(inp["gdn_conv_w"][l], np.float32)
    for i in range(6):
        pp[:, P_CONV + i * 4:P_CONV + i * 4 + 4] = cw[:, i * 128:(i + 1) * 128].T
    pp[:, P_GNW] = np.tile(inp["gdn_norm_w"][l], 2)
    pp[:, P_QNW] = np.tile(inp["attn_q_norm_w"][l], 2)
    pp[:, P_KNW] = np.tile(inp["attn_k_norm_w"][l], 2)
    pp[0:4, P_ALOG] = inp["gdn_a_log"][l]
    pp[0:4, P_DTB] = inp["gdn_dt_bias"][l]
    pp[:, P_SINK:P_SINK + 8] = np.asarray(inp["attn_sinks"][l], np.float32)[None, :]
    return pp


_NC_CACHE = {}
LAYERS_PER_LAUNCH = 1


def _get_nc(nl):
    if nl not in _NC_CACHE:
        _NC_CACHE[nl] = Builder(nl).build()
    return _NC_CACHE[nl]


def kernel(**inp):
    inp = {k: np.asarray(v) for k, v in inp.items()}
    x = inp["x"].astype(np.float32)
    depth = inp["w_in"].shape[0]
    cst = _consts()
    bband = _bias_band(inp["rel_bias"].astype(np.float32))
    xT = [np.ascontiguousarray(x[2 * c:2 * c + 2].reshape(NTOK, D).T) for c in range(NCORES)]
    nl = LAYERS_PER_LAUNCH
    for l0 in range(0, depth, nl):
        nc = _get_nc(nl)
        sl = slice(l0, l0 + nl)
        pp = np.stack([_pack_params(inp, l) for l in range(l0, l0 + nl)])
        shared = {
            "w_in": np.ascontiguousarray(inp["w_in"][sl], np.float32),
            "w_out": np.ascontiguousarray(inp["w_out"][sl], np.float32),
            "w_ff1": np.ascontiguousarray(inp["w_ff1"][sl], np.float32),
            "w_ff2": np.ascontiguousarray(inp["w_ff2"][sl], np.float32),
            "w_up": np.ascontiguousarray(inp["rwkv_w_up"][sl], np.float32),
            "a_up": np.ascontiguousarray(inp["rwkv_a_up"][sl], np.float32),
            "g_up": np.ascontiguousarray(inp["rwkv_g_up"][sl], np.float32),
            "pp": pp, "cst": cst, "bband": bband,
        }
        in_maps = [dict(shared, xT=xT[c]) for c in range(NCORES)]
        res = run_bass_kernel_spmd(nc, in_maps, core_ids=list(range(NCORES)))
        xT = [np.ascontiguousarray(res.results[c]["outT"]) for c in range(NCORES)]
    out = np.stack([xT[c].T.reshape(NSEQ, SEQ, D) for c in range(NCORES)]).reshape(16, SEQ, D)
    return out.astype(np.float32)
```

```python
import contextlib
import os
CUT = int(os.environ.get('CUT', '99'))
ALLINC = int(os.environ.get('ALLINC', '0'))
import numpy as np
import concourse.bass as bass
import concourse.mybir as mybir
from concourse.bass_utils import run_bass_kernel_spmd

F32 = mybir.dt.float32
BF16 = mybir.dt.bfloat16
AF = mybir.ActivationFunctionType
ALU = mybir.AluOpType

NCORES = 8
SEQ = 2048
NSEQ = 2
NTOK = NSEQ * SEQ
D = 1024
DIN = 2824
DFF = 4096
TT = 512
NTILE = NTOK // TT
TPS = SEQ // TT
CH = 64
NCH = TT // CH
NEG = -30000.0
DECAY_C = -0.6065306597126334

C_ID = 0
C_B1 = 128
C_B64 = 256
C_OD = 384
C_RST = 512
C_MT4 = 1024
C_MSL4 = 2048
C_I64 = 2304
C_SEL = 2560
C_BM = 2816
C_MNI = 3072
NCST = 3136
P_LN1, P_LN2, P_MU, P_W0, P_A0, P_KK, P_KA, P_RK, P_LXW, P_LXB, P_CONV = 0, 8, 16, 24, 26, 28, 30, 32, 34, 36, 38
P_GNW, P_QNW, P_KNW, P_ALOG, P_DTB, P_SINK = 62, 63, 64, 65, 66, 67
NPP = 75


class Buf:
    __slots__ = ("name", "w", "r", "pe_rows")

    def __init__(self, name):
        self.name = name
        self.w = None
        self.r = {}
        self.pe_rows = None


class View:
    __slots__ = ("ap", "keys")

    def __init__(self, ap, keys):
        self.ap = ap
        self.keys = keys

    def __getitem__(self, idx):
        return View(self.ap[idx], self.keys)

    def rr(self, pat, **kw):
        return View(self.ap.rearrange(pat, **kw), self.keys)


class Tl:
    def __init__(self, h, key):
        self.h = h
        self.key = key

    def __getitem__(self, idx):
        return View(self.h[idx], (self.key,))


def dv(ap, *keys):
    return View(ap, tuple(keys))


class Prog:
    COMPUTE = ("pe", "act", "dve", "pool")

    def __init__(self, nc, ndma=14):
        self.nc = nc
        self.E = {"pe": nc.tensor, "act": nc.scalar, "dve": nc.vector, "pool": nc.gpsimd, "sp": nc.sync}
        self.sem = {e: nc.alloc_semaphore("c_" + e) for e in self.COMPUTE}
        self.cnt = {e: 0 for e in self.COMPUTE}
        self.known = {e: {} for e in self.E}
        self.dsem = {}
        for q in ("sp", "pool"):
            self.dsem[q] = [[nc.alloc_semaphore(f"d_{q}{i}"), 0] for i in range(ndma)]
        self.drr = {q: 0 for q in self.dsem}
        self.bufs = {}
        self.nins = 0
        self.log = {e: [] for e in self.E}

    def buf(self, key):
        b = self.bufs.get(key)
        if b is None:
            b = self.bufs[key] = Buf(key)
        return b

    def _need(self, eng, ev, waits):
        if ev is None:
            return
        sem, val = ev
        if self.known[eng].get(sem, 0) >= val:
            return
        if val > waits.get(sem, 0):
            waits[sem] = val

    def _deps(self, eng, reads, writes, is_dma):
        waits = {}
        mysem = None if is_dma else self.sem[eng]
        for b in reads:
            self._need(eng, b.w, waits)
        for b in writes:
            if b.w is not None and (is_dma or b.w[0] is not mysem):
                self._need(eng, b.w, waits)
            for s, v in b.r.items():
                if is_dma or s is not mysem:
                    self._need(eng, (s, v), waits)
        return waits

    def _commit(self, ev, reads, writes):
        s, v = ev
        for b in reads:
            if b.r.get(s, 0) < v:
                b.r[s] = v
        for b in writes:
            b.w = ev
            b.r = {}

    def _bl(self, views):
        out = []
        for v in views:
            if v is None or not isinstance(v, View):
                continue
            for k in v.keys:
                out.append(self.buf(k))
        return out

    def op(self, eng, fn, reads=(), writes=(), inc=True, extra=()):
        if ALLINC or eng == "pe":
            inc = True
        reads = self._bl(reads)
        writes = self._bl(writes)
        for b in reads:
            if isinstance(b.name, str) and b.name.startswith("pb") and b not in writes:
                writes.append(b)
        waits = self._deps(eng, reads, writes, False)
        for ev in extra:
            self._need(eng, ev, waits)
        e = self.E[eng]
        for s, v in waits.items():
            self.known[eng][s] = v
            e.wait_ge(s, v)
            self.nins += 1
        sem = self.sem[eng]
        ins = fn(e)
        self.nins += 1
        self.log[eng].append((list(waits.items()), (sem, 1) if inc else None))
        if inc:
            self.cnt[eng] += 1
            ev = (sem, self.cnt[eng])
            ins.then_inc(sem, 1)
        else:
            ev = (sem, self.cnt[eng] + 1)
        self._commit(ev, reads, writes)

    def dma(self, q, out, in_):
        reads = self._bl([in_])
        writes = self._bl([out])
        waits = self._deps(q, reads, writes, True)
        pool = self.dsem[q]
        i = self.drr[q]
        self.drr[q] = (i + 1) % len(pool)
        sem, n = pool[i]
        if n > 0:
            self._need(q, (sem, 16 * n), waits)
        pool[i][1] = n + 1
        ev = (sem, 16 * (n + 1))
        e = self.E[q]
        for s, v in waits.items():
            self.known[q][s] = v
            e.wait_ge(s, v)
            self.nins += 1
        e.dma_start(out=out.ap, in_=in_.ap).then_inc(sem, 16)
        self.nins += 1
        self.log[q].append((list(waits.items()), (sem, 16)))
        self._commit(ev, reads, writes)

    def simulate(self):
        pos = {e: 0 for e in self.log}
        val = {}
        prog = True
        while prog:
            prog = False
            for e, lst in self.log.items():
                while pos[e] < len(lst):
                    waits, inc = lst[pos[e]]
                    if all(val.get(s_, 0) >= v for s_, v in waits):
                        if inc is not None:
                            val[inc[0]] = val.get(inc[0], 0) + inc[1]
                        pos[e] += 1
                        prog = True
                    else:
                        break
        stuck = {e: (pos[e], len(l)) for e, l in self.log.items() if pos[e] < len(l)}
        for e in stuck:
            waits, inc = self.log[e][pos[e]]
            print("STUCK", e, pos[e], [(s_.name, v, val.get(s_, 0)) for s_, v in waits])
        return stuck

    def barrier(self):
        targets = [(self.sem[e], self.cnt[e]) for e in self.COMPUTE if self.cnt[e] > 0]
        for q in self.dsem:
            targets += [(s_, 16 * n) for s_, n in self.dsem[q] if n > 0]
        for eng, e in self.E.items():
            wl = []
            for s_, v in targets:
                if self.known[eng].get(s_, 0) < v:
                    self.known[eng][s_] = v
                    e.wait_ge(s_, v)
                    wl.append((s_, v))
                    self.nins += 1
            self.log[eng].append((wl, None))

    def wait_all_dma(self, q):
        e = self.E[q]
        for s, n in self.dsem[q]:
            if n > 0:
                e.wait_ge(s, 16 * n)

    def _pe_rows(self, out, lhsT):
        b0 = lhsT.ap.base_partition()
        k = lhsT.ap.partition_size()
        rows = (b0 // 32, (b0 + k + 31) // 32)
        extra = []
        for key in out.keys:
            b = self.buf(key)
            pr = b.pe_rows
            if pr is not None and (pr[1] <= rows[0] or rows[1] <= pr[0]) and b.w is not None and b.w[0] is self.sem["pe"]:
                extra.append(b.w)
            b.pe_rows = rows
        return extra

    def mm(self, out, lhsT, rhs, start=True, stop=True, inc=True):
        extra = self._pe_rows(out, lhsT)
        self.op("pe", lambda e: e.matmul(out.ap, lhsT.ap, rhs.ap, start=start, stop=stop),
                reads=[lhsT, rhs], writes=[out], inc=inc, extra=extra)

    def tr(self, out, in_, ident, inc=True):
        extra = self._pe_rows(out, in_)
        if os.environ.get("TRMODE", "tr") == "mm":
            self.op("pe", lambda e: e.matmul(out.ap, in_.ap, ident.ap, start=True, stop=True), reads=[in_, ident], writes=[out], extra=extra)
        else:
            self.op("pe", lambda e: e.transpose(out.ap, in_.ap, ident.ap), reads=[in_, ident], writes=[out], extra=extra)

    def act(self, out, in_, func, bias=None, scale=None, eng="act"):
        kw = {}
        rd = [in_]
        if bias is not None:
            if isinstance(bias, View):
                kw["bias"] = bias.ap
                rd.append(bias)
            else:
                kw["bias"] = bias
        if scale is not None:
            if isinstance(scale, View):
                kw["scale"] = scale.ap
                rd.append(scale)
            else:
                kw["scale"] = scale
        self.op("act", lambda e: e.activation(out.ap, in_.ap, func, **kw), reads=rd, writes=[out])

    def tt(self, eng, out, a, b, op):
        self.op(eng, lambda e: e.tensor_tensor(out.ap, a.ap, b.ap, op), reads=[a, b], writes=[out])

    def ts(self, eng, out, a, s1, op0, s2=None, op1=None):
        rd = [a]
        v1 = s1
        if isinstance(s1, View):
            rd.append(s1)
            v1 = s1.ap
        v2 = s2
        if isinstance(s2, View):
            rd.append(s2)
            v2 = s2.ap
        if op1 is None:
            self.op(eng, lambda e: e.tensor_scalar(out.ap, a.ap, v1, None, op0), reads=rd, writes=[out])
        else:
            self.op(eng, lambda e: e.tensor_scalar(out.ap, a.ap, v1, v2, op0, op1), reads=rd, writes=[out])

    def stt(self, eng, out, a, s, b, op0, op1):
        eng = "dve"
        rd = [a, b]
        sv = s
        if isinstance(s, View):
            rd.append(s)
            sv = s.ap
        self.op(eng, lambda e: e.scalar_tensor_tensor(out.ap, a.ap, sv, b.ap, op0, op1), reads=rd, writes=[out])

    def cp(self, eng, out, in_):
        if eng == "act":
            self.op("act", lambda e: e.copy(out.ap, in_.ap), reads=[in_], writes=[out])
        else:
            self.op(eng, lambda e: e.tensor_copy(out.ap, in_.ap), reads=[in_], writes=[out])

    def memset(self, eng, out, val):
        self.op(eng, lambda e: e.memset(out.ap, val), writes=[out])

    def recip(self, out, in_):
        self.op("dve", lambda e: e.reciprocal(out.ap, in_.ap), reads=[in_], writes=[out])

    def scan(self, out, d0, d1):
        self.op("dve", lambda e: e.tensor_tensor_scan(out.ap, d0.ap, d1.ap, 0.0, ALU.mult, ALU.add),
                reads=[d0, d1], writes=[out])


class Builder:
    def __init__(self, nlayers, debug=False, stop_after=None):
        self.stop_after = stop_after
        self.mix_parts = ("rwkv", "gdn", "attn")
        self.mix_tiles = TPS
        self.mix_nseq = NSEQ
        if stop_after and stop_after.startswith("mix:"):
            f = stop_after.split(":")
            self.mix_parts = tuple(f[1].split(","))
            self.mix_tiles = int(f[2])
            self.mix_nseq = int(f[3])
        self.L = nlayers
        nc = self.nc = bass.Bass("TRN2", target_bir_lowering=False)
        self.p = Prog(nc)
        L = nlayers
        di = lambda n, s, dt=F32: nc.dram_tensor(n, s, dt, kind="ExternalInput")
        self.xT = di("xT", [D, NTOK])
        self.w_in = di("w_in", [L, D, DIN])
        self.w_out = di("w_out", [L, D, D])
        self.w_ff1 = di("w_ff1", [L, D, DFF])
        self.w_ff2 = di("w_ff2", [L, DFF, D])
        self.w_up = di("w_up", [L, 64, 256])
        self.a_up = di("a_up", [L, 64, 256])
        self.g_up = di("g_up", [L, 128, 256])
        self.pp = di("pp", [L, 128, NPP])
        self.cst = di("cst", [128, NCST])
        self.bband = di("bband", [128, 2 * 8 * 128])
        self.outT = nc.dram_tensor("outT", [D, NTOK], F32, kind="ExternalOutput")
        self.xa = nc.dram_tensor("xa_d", [D, NTOK], F32)
        kd = "ExternalOutput" if debug else "Internal"
        self.xb = nc.dram_tensor("xb_d", [D, NTOK], F32, kind=kd)
        self.z_d = nc.dram_tensor("z_d", [DIN, NTOK], F32, kind=kd)
        self.vtm_d = nc.dram_tensor("vtm_d", [NTOK, 128], F32, kind=kd)
        self.y_d = nc.dram_tensor("y_d", [D, NTOK], BF16, kind=kd)
        self._uid = 0

    def sb(self, st, shape, dt=F32, name=None):
        self._uid += 1
        name = name or f"t{self._uid}"
        h = st.enter_context(self.nc.sbuf_tensor(f"{name}_{self._uid}", shape, dt))
        return Tl(h, f"{name}_{self._uid}")

    def ps(self, st, n):
        out = []
        for i in range(n):
            self._uid += 1
            h = st.enter_context(self.nc.psum_tensor(f"ps{self._uid}", [128, 512], F32))
            out.append(h)
        return out

    def build(self):
        p = self.p
        with contextlib.ExitStack() as st:
            self.cs = self.sb(st, [128, NCST], F32, "cst")
            p.dma("sp", self.cs[:], dv(self.cst.ap(), "cst_d"))
            self.ppt = self.sb(st, [128, NPP], F32, "pp")
            self.der = self.sb(st, [128, 12], F32, "der")
            cur = self.xT
            for l in range(self.L):
                p.dma("sp", self.ppt[:], dv(self.pp.ap()[l], "pp_d"))
                p.ts("dve", self.der[:, 0:1], self.ppt[:, P_QNW:P_QNW + 1], 0.125, ALU.mult)
                p.act(self.der[0:4, 1:2], self.ppt[0:4, P_ALOG:P_ALOG + 1], AF.Exp)
                p.ts("dve", self.der[0:4, 1:2], self.der[0:4, 1:2], -1.0, ALU.mult)
                p.act(self.der[:, 2:10], self.ppt[:, P_SINK:P_SINK + 8], AF.Exp)
                last = (l == self.L - 1)
                self.phase_inproj(l, cur)
                p.barrier()
                if self.stop_after == "inproj":
                    break
                self.phase_mix(l)
                p.barrier()
                if self.stop_after and self.stop_after.startswith("mix"):
                    break
                self.phase_outproj(l, cur, self.xb)
                p.barrier()
                if self.stop_after == "outproj":
                    break
                dst = self.outT if last else self.xa
                self.phase_ffn(l, self.xb, dst)
                p.barrier()
                cur = self.xa
            p.wait_all_dma("pool")
            p.wait_all_dma("sp")
        return self.nc

    def rms_tile(self, xt, ht, lncol, sq2, rstd, pbank, pkey):
        p = self.p
        ones = self.cs[:, C_OD:C_OD + 128]
        pv = dv(pbank[:, :], pkey)
        for c in range(8):
            s = sq2[c % 2]
            p.act(s[:], xt[:, c, :], AF.Square)
            p.mm(pv, ones, s[:], start=(c == 0), stop=(c == 7), inc=(c == 7) or True)
        p.act(rstd[:], pv, AF.Sqrt, bias=1e-6)
        p.recip(rstd[:], rstd[:])
        for c in range(8):
            eng = "dve" if c % 2 == 0 else "pool"
            p.stt(eng, ht[:, c, :], xt[:, c, :], self.ppt[:, lncol + c: lncol + c + 1], rstd[:], ALU.mult, ALU.mult)

    def phase_inproj(self, l, xsrc):
        p = self.p
        with contextlib.ExitStack() as st:
            wb = self.sb(st, [128, 8, DIN], BF16, "winb")
            for c in range(8):
                p.dma("pool", wb[:, c, :], dv(self.w_in.ap()[l, c * 128:(c + 1) * 128, :], "w_in_d"))
            xts = [self.sb(st, [128, 8, TT], F32, "xt") for _ in range(2)]
            ht = self.sb(st, [128, 8, TT], BF16, "ht")
            sq2 = [self.sb(st, [128, TT], F32, "sq") for _ in range(2)]
            rstd = self.sb(st, [128, TT], F32, "rstd")
            stg = [self.sb(st, [128, 2, TT], F32, "stg") for _ in range(3)]
            vst = self.sb(st, [128, 4, 128], F32, "vst")
            banks = self.ps(st, 8)
            xsrc_v = xsrc.ap().rearrange("(c p) t -> p c t", p=128)
            ngrp = 22
            for j in range(NTILE):
                xt = xts[j % 2]
                t0 = j * TT
                for hh in range(2):
                    p.dma("sp", xt[:, hh * 4:(hh + 1) * 4, :], dv(xsrc_v[:, hh * 4:(hh + 1) * 4, t0:t0 + TT], ("x", id(xsrc), j)))
                self.rms_tile(xt, ht, P_LN1, sq2, rstd, banks[7], "pb7")
                gi = 0
                for g in range(ngrp):
                    bk = banks[g % 6]
                    pv = dv(bk[:, :], f"pb{g % 6}")
                    for c in range(8):
                        p.mm(pv, wb[:, c, g * 128:(g + 1) * 128], ht[:, c, :], start=(c == 0), stop=(c == 7), inc=(c == 7))
                    s = stg[(g // 2) % 3]
                    if g % 2 == 0:
                        p.cp("act", s[:, 0, :], pv)
                    else:
                        p.cp("dve", s[:, 1, :], pv)
                    if g % 2 == 1 or g == ngrp - 1:
                        g0 = g - (g % 2)
                        ng = g - g0 + 1
                        dst = self.z_d.ap()[g0 * 128:(g0 + ng) * 128, t0:t0 + TT].rearrange("(g p) t -> p g t", p=128)
                        p.dma("pool", dv(dst, ("z", g0, j), ("z", g0 + 1, j)), s[:, 0:ng, :])
                pv = dv(banks[6][:, :], "pb6")
                for q in range(4):
                    for c in range(8):
                        p.mm(dv(banks[6][:, q * 128:(q + 1) * 128], "pb6"), ht[:, c, q * 128:(q + 1) * 128],
                             wb[:, c, 2696:2824], start=(c == 0), stop=(c == 7), inc=(c == 7))
                p.cp("act", vst[:].rr("p a b -> p (a b)"), pv)
                dst = self.vtm_d.ap()[t0:t0 + TT, :].rearrange("(q p) f -> p q f", p=128)
                p.dma("pool", dv(dst, ("vtm", j)), vst[:])

    def dplr_setup(self, st):
        s = {}
        s["tmV"] = self.sb(st, [64, NCH, 256], F32, "tmV")
        s["tmB"] = self.sb(st, [64, NCH, 256], F32, "tmB")
        s["tmK"] = self.sb(st, [64, NCH, 256], F32, "tmK")
        s["GTs"] = [self.sb(st, [64, 4, 256], F32, "GTs") for _ in range(2)]
        s["M"] = [self.sb(st, [64, 2, 256], F32, "M") for _ in range(2)]
        s["Tt"] = self.sb(st, [64, NCH, 256], F32, "Tt")
        s["DT"] = self.sb(st, [64, 256], F32, "DT")
        s["DTm"] = self.sb(st, [64, 1024], F32, "DTm")
        s["gcol"] = self.sb(st, [64, 32], F32, "gcol")
        s["Hs"] = [self.sb(st, [128, 64], F32, "Hs") for _ in range(2)]
        s["Xs"] = self.sb(st, [64, 256], F32, "Xs")
        s["Us"] = self.sb(st, [64, 256], F32, "Us")
        return s

    def dplr_tile(self, s, banks, arT, bT, kT, vT, pc, H, yout, arG=None, bG=None, kG=None, gam=None):
        p = self.p
        cs = self.cs
        ident = cs[:, C_ID:C_ID + 128]
        bk = lambda i: dv(banks[i][:, :], f"pb{i}")
        bks = lambda i, sl: dv(banks[i][sl], f"pb{i}")
        scalar_mode = gam is not None
        if arG is None:
            arG, bG, kG = arT, bT, kT
        if scalar_mode:
            for c in range(NCH):
                p.tr(bks(7, (slice(0, 64), slice(c * 4, c * 4 + 4))), gam[0:4, c * 64:(c + 1) * 64], cs[0:4, C_ID:C_ID + 4], inc=(c == NCH - 1))
            p.cp("act", s["gcol"][:], bks(7, (slice(0, 64), slice(0, 32))))
        for ti, (src, dst) in enumerate(((vT, s["tmV"]), (bT, s["tmB"]), (kT, s["tmK"]))):
            for cp2 in range(NCH // 2):
                b = 2 + (ti * (NCH // 2) + cp2) % 2
                for cc in range(2):
                    c = cp2 * 2 + cc
                    for hp in range(2):
                        p.tr(bks(b, (slice(0, 64), slice(cc * 256 + hp * 128, cc * 256 + hp * 128 + 128))),
                             src[hp][:, c * 64:(c + 1) * 64], ident, inc=(cc == 1 and hp == 1))
                eng = "act" if (cp2 % 2 == 0) else "dve"
                p.cp(eng, dst[:, cp2 * 2:cp2 * 2 + 2, :].rr("p a b -> p (a b)"), bks(b, (slice(0, 64), slice(0, 512))))
        for c in range(NCH):
            G = s["GTs"][c % 2]
            csl = slice(c * 64, (c + 1) * 64)
            for h in range(4):
                hp, sl = h // 2, slice((h % 2) * 64, (h % 2) * 64 + 64)
                gb = 4 + h // 2
                o = (h % 2) * 256
                p.mm(bks(gb, (slice(0, 64), slice(o, o + 128))), bG[hp][sl, csl], arG[hp][sl, c, :], inc=False)
                p.mm(bks(gb, (slice(0, 64), slice(o + 128, o + 256))), kG[hp][sl, csl], arG[hp][sl, c, :], inc=(h % 2 == 1))
            if scalar_mode:
                for h in range(4):
                    p.mm(bks(6, (slice(0, 64), slice(h * 64, h * 64 + 64))), cs[0:4, C_SEL + h * 64:C_SEL + h * 64 + 64],
                         gam[0:4, csl], inc=(h == 3))
                for h in range(4):
                    p.stt("dve", s["DT"][:, h * 64:h * 64 + 64], bks(6, (slice(0, 64), slice(h * 64, h * 64 + 64))),
                          s["gcol"][:, c * 4 + h:c * 4 + h + 1], cs[0:64, C_MNI:C_MNI + 64], ALU.subtract, ALU.add)
                p.act(s["DT"][:], s["DT"][:], AF.Exp)
                dt4 = View(s["DT"][:].ap.rearrange("p (h t) -> p h t", h=4).unsqueeze(2).broadcast_to([64, 4, 4, 64]), s["DT"][:].keys)
                p.tt("dve", s["DTm"][:].rr("p (h b t) -> p h b t", h=4, b=4), cs[0:64, C_MT4:C_MT4 + 1024].rr("p (h b t) -> p h b t", h=4, b=4),
                     dt4, ALU.mult)
                mk = lambda half: s["DTm"][:, half * 512:half * 512 + 512]
            else:
                mk = lambda half: cs[0:64, C_MT4 + half * 512: C_MT4 + half * 512 + 512]
            for half in range(2):
                p.tt("dve", G[:, half * 2:half * 2 + 2, :].rr("p a b -> p (a b)"), bks(4 + half, (slice(0, 64), slice(0, 512))),
                     mk(half), ALU.mult)
            M0 = s["M"][0]
            p.cp("act", M0[:, 1, :].rr("p (h t) -> p h t", h=4), G[:, :, 0:64])
            for h in range(4):
                p.tr(bks(6, (slice(0, 64), slice(h * 64, h * 64 + 64))), G[:, h, 0:64], cs[0:64, C_ID:C_ID + 64], inc=(h == 3))
            p.cp("dve", M0[:, 0, :], bks(6, (slice(0, 64), slice(0, 256))))
            Tt = s["Tt"]
            p.tt("dve", Tt[:, c, :], M0[:, 1, :], cs[0:64, C_I64:C_I64 + 256], ALU.add)
            cur = 0
            for lvl in range(5):
                Mc = s["M"][cur]
                Mn = s["M"][1 - cur]
                lastl = (lvl == 4)
                for h in range(4):
                    hs = slice(h * 64, h * 64 + 64)
                    p.mm(bks(7, (slice(0, 64), hs)), Mc[:, 1, hs], Mc[:, 0, hs], inc=(lastl and h == 3))
                if not lastl:
                    for h in range(4):
                        hs = slice(h * 64, h * 64 + 64)
                        p.mm(bks(7, (slice(0, 64), slice(256 + h * 64, 256 + h * 64 + 64))), Mc[:, 0, hs], Mc[:, 1, hs], inc=(h == 3))
                    p.cp("act", Mn[:].rr("p a b -> p (a b)"), bks(7, (slice(0, 64), slice(0, 512))))
                else:
                    p.cp("act", Mn[:, 0, :], bks(7, (slice(0, 64), slice(0, 256))))
                for h in range(4):
                    hs = slice(h * 64, h * 64 + 64)
                    p.mm(bks(6, (slice(0, 64), slice(256 + h * 64, 256 + h * 64 + 64))), Mn[:, 0, hs], Tt[:, c, hs], inc=(h == 3))
                p.tt("dve", Tt[:, c, :], Tt[:, c, :], bks(6, (slice(0, 64), slice(256, 512))), ALU.add)
                cur = 1 - cur
            Xs, Us = s["Xs"], s["Us"]
            for h in range(4):
                hp, sl = h // 2, slice((h % 2) * 64, (h % 2) * 64 + 64)
                hs = slice(h * 64, h * 64 + 64)
                o = bks(0, (slice(0, 64), hs))
                p.mm(o, arT[hp][sl, c, 0:64], H[hp][sl, :], start=True, stop=False, inc=False)
                p.mm(o, G[:, h, 128:192], s["tmV"][:, c, hs], start=False, stop=True, inc=(h == 3))
            p.cp("act", Xs[:], bks(0, (slice(0, 64), slice(0, 256))))
            for h in range(4):
                hs = slice(h * 64, h * 64 + 64)
                p.mm(bks(0, (slice(0, 64), slice(256 + h * 64, 256 + h * 64 + 64))), Tt[:, c, hs], Xs[:, hs], inc=(h == 3))
            p.cp("dve", Us[:], bks(0, (slice(0, 64), slice(256, 512))))
            for h in range(4):
                hp, sl = h // 2, slice((h % 2) * 64, (h % 2) * 64 + 64)
                hs = slice(h * 64, h * 64 + 64)
                yb = 1 if hp == 0 else 3
                o = bks(yb, (sl, csl))
                p.mm(o, H[hp][sl, :], arT[hp][sl, c, 64:128], start=True, stop=False, inc=False)
                p.mm(o, Us[:, hs], G[:, h, 64:128], start=False, stop=False, inc=False)
                p.mm(o, s["tmV"][:, c, hs], G[:, h, 192:256], start=False, stop=True, inc=(h % 2 == 1))
            if scalar_mode:
                for hp in range(2):
                    p.act(s["Hs"][hp][:], H[hp][:], AF.Copy, scale=pc(hp, c))
                Hsrc = s["Hs"]
            else:
                Hsrc = H
            for h in range(4):
                hp, sl = h // 2, slice((h % 2) * 64, (h % 2) * 64 + 64)
                hs = slice(h * 64, h * 64 + 64)
                o = bks(2, (sl, slice(hp * 64, hp * 64 + 64)))
                p.mm(o, cs[sl, C_ID + (h % 2) * 64: C_ID + (h % 2) * 64 + 64], Hsrc[hp][sl, :], start=True, stop=False, inc=False)
                p.mm(o, s["tmB"][:, c, hs], Us[:, hs], start=False, stop=False, inc=False)
                p.mm(o, s["tmK"][:, c, hs], s["tmV"][:, c, hs], start=False, stop=True, inc=(h % 2 == 1))
            for hp in range(2):
                if scalar_mode:
                    p.cp("act", H[hp][:], bks(2, (slice(0, 128), slice(hp * 64, hp * 64 + 64))))
                else:
                    p.act(H[hp][:], bks(2, (slice(0, 128), slice(hp * 64, hp * 64 + 64))), AF.Copy, scale=pc(hp, c))
        for hp in range(2):
            yb = 1 if hp == 0 else 3
            p.cp("act" if hp == 0 else "dve", yout[hp][:], bk(yb))

    def phase_mix(self, l):
        p = self.p
        cs = self.cs
        pp = self.ppt
        with contextlib.ExitStack() as st:
            banks = self.ps(st, 8)
            bk = lambda i: dv(banks[i][:, :], f"pb{i}")
            bks = lambda i, sl: dv(banks[i][sl], f"pb{i}")
            self.bm = self.sb(st, [128, 2 * 8 * 128], F32, "biasm")
            p.dma("sp", self.bm[:], dv(self.bband.ap(), "bb_d"))
            for kind in range(2):
                for h in range(8):
                    o = self.bm[:, (kind * 8 + h) * 128:(kind * 8 + h + 1) * 128]
                    p.tt("dve", o, o, self.cs[:, C_BM + kind * 128: C_BM + (kind + 1) * 128], ALU.add)
            s = self.dplr_setup(st)
            W = [self.sb(st, [128, TT + 3], F32, "W") for _ in range(34)]
            arT = [self.sb(st, [128, NCH, 128], F32, "arT") for _ in range(2)]
            arG = [self.sb(st, [128, NCH, 128], F32, "arG") for _ in range(2)]
            Hst = [self.sb(st, [128, 64], F32, "H") for _ in range(2)]
            sm = [self.sb(st, [4, TT], F32, "sm") for _ in range(11)]
            yb16 = [self.sb(st, [128, TT], BF16, "yb") for _ in range(2)]
            wup = self.sb(st, [128, 256], F32, "wup")
            aup = self.sb(st, [128, 256], F32, "aup")
            gup = self.sb(st, [128, 256], F32, "gup")
            p.dma("sp", wup[0:64, :], dv(self.w_up.ap()[l], "wup_d"))
            p.dma("sp", aup[64:128, :], dv(self.a_up.ap()[l], "aup_d"))
            p.dma("sp", gup[:, :], dv(self.g_up.ap()[l], "gup_d"))
            b1 = cs[:, C_B1:C_B1 + 128]
            b64 = cs[:, C_B64:C_B64 + 128]
            rst = cs[:, C_RST:C_RST + TT]
            zd = self.z_d.ap()
            col = lambda c: pp[:, c:c + 1]

            def load_halo(tile, row0, nrows, t0, j, halo, first, gkeys, prow=0):
                rows = slice(prow, prow + nrows)
                if first:
                    if halo > 0:
                        p.memset("pool", tile[rows, 0:halo], 0.0)
                    p.dma("sp", tile[rows, halo:halo + TT], dv(zd[row0:row0 + nrows, t0:t0 + TT], *[(("z",) + (g, j)) for g in gkeys]))
                else:
                    p.dma("sp", tile[rows, 0:halo + TT], dv(zd[row0:row0 + nrows, t0 - halo:t0 + TT],
                                                           *([(("z",) + (g, j)) for g in gkeys] + [(("z",) + (g, j - 1)) for g in gkeys])))

            def zkey(row0):
                g = row0 // 128
                return [g - (g % 2), g - (g % 2) + 1]

            def l2rn(x, tmp, tmp2, bank, eps, blk):
                p.act(tmp[:, 0:TT], x, AF.Square)
                p.mm(bk(bank), blk, tmp[:, 0:TT])
                p.act(tmp2[:, 0:TT], bk(bank), AF.Sqrt, bias=eps)
                p.recip(tmp2[:, 0:TT], tmp2[:, 0:TT])

            for sq in range(self.mix_nseq):
                for hp in range(2):
                    p.memset("pool", Hst[hp][:], 0.0)
                for jt in range(self.mix_tiles if "rwkv" in self.mix_parts else 0):
                    j = sq * TPS + jt
                    t0 = j * TT
                    first = (jt == 0)
                    Z = W[0:8]
                    XS = W[8:16]
                    for role in range(8):
                        load_halo(Z[role], role * 128, 128, t0, j, 1, first, zkey(role * 128))
                        eng = "dve" if role % 2 == 0 else "pool"
                        p.tt(eng, W[16][:, 0:TT], Z[role][:, 0:TT], Z[role][:, 1:TT + 1], ALU.subtract)
                        p.stt(eng, XS[role][:, 0:TT], W[16][:, 0:TT], col(P_MU + role), Z[role][:, 1:TT + 1], ALU.mult, ALU.add)
                    xr, xk, xv, xdd, xdg = XS[0:2], XS[2:4], XS[4:6], XS[6], XS[7]
                    p.act(xdd[0:64, 0:TT], xdd[0:64, 0:TT], AF.Tanh)
                    p.act(xdg[:, 0:TT], xdg[:, 0:TT], AF.Sigmoid)
                    lw, iclr, gate, kk = W[0:2], W[2:4], W[4:6], W[6:8]
                    cum, Pin, Pex, Pinv = W[17:19], W[19:21], W[21:23], W[23:25]
                    bT, kT, bon = W[25:27], W[27:29], W[29:31]
                    yo = W[31:33]
                    tmp = W[16]
                    tmp2 = W[33]
                    for hp in range(2):
                        hsl = slice(hp * 128, hp * 128 + 128)
                        p.mm(bk(0), wup[0:64, hsl], xdd[0:64, 0:TT])
                        p.act(lw[hp][:, 0:TT], bk(0), AF.Sigmoid, bias=col(P_W0 + hp))
                        p.ts("dve", lw[hp][:, 0:TT], lw[hp][:, 0:TT], DECAY_C, ALU.mult)
                        p.mm(bk(1), aup[64:128, hsl], xdd[64:128, 0:TT])
                        p.act(iclr[hp][:, 0:TT], bk(1), AF.Sigmoid, bias=col(P_A0 + hp))
                        p.mm(bk(0), gup[:, hsl], xdg[:, 0:TT])
                        p.cp("act", gate[hp][:, 0:TT], bk(0))
                        p.ts("dve", kk[hp][:, 0:TT], xk[hp][:, 0:TT], col(P_KK + hp), ALU.mult)
                        l2rn(kk[hp][:, 0:TT], tmp, tmp2, 1, 1e-6, b1)
                        p.tt("dve", kk[hp][:, 0:TT], kk[hp][:, 0:TT], tmp2[:, 0:TT], ALU.mult)
                        p.ts("pool", tmp[:, 0:TT], iclr[hp][:, 0:TT], -1.0, ALU.add, col(P_KA + hp), ALU.mult)
                        p.stt("pool", xk[hp][:, 0:TT], tmp[:, 0:TT], 1.0, xk[hp][:, 0:TT], ALU.add, ALU.mult)
                        p.scan(cum[hp][:, 0:TT], rst, lw[hp][:, 0:TT])
                        p.act(Pin[hp][:, 0:TT], cum[hp][:, 0:TT], AF.Exp)
                        p.act(Pinv[hp][:, 0:TT], cum[hp][:, 0:TT], AF.Exp, scale=-1.0)
                        p.tt("dve", tmp[:, 0:TT], cum[hp][:, 0:TT], lw[hp][:, 0:TT], ALU.subtract)
                        p.act(Pex[hp][:, 0:TT], tmp[:, 0:TT], AF.Exp)
                        a3 = arT[hp][:]
                        v3 = lambda t: t[:, 0:TT].rr("p (c t) -> p c t", t=64)
                        p.stt("dve", a3[:, :, 0:64], v3(kk[hp]), -1.0, v3(Pex[hp]), ALU.mult, ALU.mult)
                        p.tt("pool", a3[:, :, 64:128], v3(xr[hp]), v3(Pin[hp]), ALU.mult)
                        p.tt("dve", tmp[:, 0:TT], kk[hp][:, 0:TT], iclr[hp][:, 0:TT], ALU.mult)
                        p.tt("dve", bT[hp][:, 0:TT], tmp[:, 0:TT], Pinv[hp][:, 0:TT], ALU.mult)
                        p.tt("pool", kT[hp][:, 0:TT], xk[hp][:, 0:TT], Pinv[hp][:, 0:TT], ALU.mult)
                        p.stt("dve", tmp[:, 0:TT], xr[hp][:, 0:TT], col(P_RK + hp), xk[hp][:, 0:TT], ALU.mult, ALU.mult)
                        p.mm(bk(1), b1, tmp[:, 0:TT])
                        p.tt("dve", bon[hp][:, 0:TT], bk(1), xv[hp][:, 0:TT], ALU.mult)
                    pc = lambda hp, c: Pin[hp][:, c * 64 + 63: c * 64 + 64]
                    self.dplr_tile(s, banks, arT, [t[:, 0:TT] for t in bT], [t[:, 0:TT] for t in kT],
                                   [t[:, 0:TT] for t in xv], pc, Hst, [t[:, 0:TT] for t in yo])
                    for hp in range(2):
                        y = yo[hp][:, 0:TT]
                        p.mm(bk(0), b64, y)
                        p.tt("dve", y, y, bk(0), ALU.subtract)
                        l2rn(y, tmp, tmp2, 1, 64e-5, b64)
                        p.tt("dve", y, y, tmp2[:, 0:TT], ALU.mult)
                        p.ts("dve", y, y, col(P_LXW + hp), ALU.mult, col(P_LXB + hp), ALU.add)
                        p.tt("pool", y, y, bon[hp][:, 0:TT], ALU.add)
                        p.tt("dve", yb16[hp][:], y, gate[hp][:, 0:TT], ALU.mult)
                        p.dma("pool", dv(self.y_d.ap()[hp * 128:(hp + 1) * 128, t0:t0 + TT], ("y", hp, j)), yb16[hp][:])
                for hp in range(2):
                    p.memset("pool", Hst[hp][:], 0.0)
                for jt in range(self.mix_tiles if "gdn" in self.mix_parts else 0):
                    j = sq * TPS + jt
                    t0 = j * TT
                    first = (jt == 0)
                    Z = W[0:6]
                    CV = W[6:12]
                    gt = W[12:14]
                    for i in range(6):
                        r0 = 1024 + i * 128
                        load_halo(Z[i], r0, 128, t0, j, 3, first, zkey(r0))
                        eng = "dve" if i % 2 == 0 else "pool"
                        cw = lambda k: col(P_CONV + i * 4 + k)
                        o = CV[i][:, 0:TT]
                        p.ts(eng, o, Z[i][:, 0:TT], cw(0), ALU.mult)
                        for k in range(1, 4):
                            p.stt(eng, o, Z[i][:, k:k + TT], cw(k), o, ALU.mult, ALU.add)
                        p.act(o, o, AF.Silu)
                    for hp in range(2):
                        r0 = 1792 + hp * 128
                        load_halo(gt[hp], r0, 128, t0, j, 0, True, zkey(r0))
                        p.act(gt[hp][:, 0:TT], gt[hp][:, 0:TT], AF.Silu)
                    bl, al = sm[0], sm[1]
                    p.dma("sp", bl[:], dv(zd[2048:2052, t0:t0 + TT], ("z", 16, j), ("z", 17, j)))
                    p.dma("sp", al[:], dv(zd[2052:2056, t0:t0 + TT], ("z", 16, j), ("z", 17, j)))
                    beta, xx, ax, ee, gg, gam, E1, nb, BE1n, Ff, BF = sm[0:11]
                    p.act(beta[:], bl[:], AF.Sigmoid)
                    p.ts("dve", xx[:], al[:], pp[0:4, P_DTB:P_DTB + 1], ALU.add)
                    p.act(ax[:], xx[:], AF.Abs)
                    p.act(ee[:], ax[:], AF.Exp, scale=-1.0)
                    p.act(ee[:], ee[:], AF.Ln, bias=1.0)
                    p.stt("dve", gg[:], xx[:], 0.0, ee[:], ALU.max, ALU.add)
                    p.ts("dve", gg[:], gg[:], self.der[0:4, 1:2], ALU.mult)
                    p.scan(gam[:], cs[0:4, C_RST:C_RST + TT], gg[:])
                    p.act(E1[:], gam[:], AF.Exp)
                    p.ts("dve", nb[:], beta[:], -1.0, ALU.mult)
                    p.tt("dve", BE1n[:], nb[:], E1[:], ALU.mult)
                    g3 = gam[:].rr("p (c t) -> p c t", t=64)
                    gend = View(g3.ap[:, :, 63:64].broadcast_to([4, NCH, 64]), g3.keys)
                    p.tt("dve", Ff[:].rr("p (c t) -> p c t", t=64), gend, g3, ALU.subtract)
                    p.act(Ff[:], Ff[:], AF.Exp)
                    p.tt("dve", BF[:], beta[:], Ff[:], ALU.mult)
                    cq, ck, cv = CV[0:2], CV[2:4], CV[4:6]
                    bT, kT, yo = W[14:16], W[16:18], W[18:20]
                    tmp, tmp2 = W[20], W[21]
                    pct = W[22]
                    kG = W[23:25]
                    for hp in range(2):
                        l2rn(cq[hp][:, 0:TT], tmp, tmp2, 0, 1e-6, b1)
                        p.stt("dve", cq[hp][:, 0:TT], cq[hp][:, 0:TT], 0.125, tmp2[:, 0:TT], ALU.mult, ALU.mult)
                        l2rn(ck[hp][:, 0:TT], tmp, tmp2, 1, 1e-6, b1)
                        p.tt("dve", ck[hp][:, 0:TT], ck[hp][:, 0:TT], tmp2[:, 0:TT], ALU.mult)
                        a3 = arT[hp][:]
                        g3a = arG[hp][:]
                        v3 = lambda t: t[:, 0:TT].rr("p (c t) -> p c t", t=64)
                        pv3 = lambda b: bk(b).rr("p (c t) -> p c t", t=64)

                        def bcast(bank, fac):
                            for hh in range(2):
                                h = hp * 2 + hh
                                p.mm(bks(bank, (slice(hh * 64, hh * 64 + 64), slice(0, TT))), cs[0:4, C_SEL + h * 64: C_SEL + h * 64 + 64],
                                     fac[:], inc=(hh == 1))
                        bcast(0, nb)
                        p.tt("dve", g3a[:, :, 0:64], v3(ck[hp]), pv3(0), ALU.mult)
                        p.cp("pool", g3a[:, :, 64:128], v3(cq[hp]))
                        bcast(1, BE1n)
                        p.tt("dve", a3[:, :, 0:64], v3(ck[hp]), pv3(1), ALU.mult)
                        bcast(0, E1)
                        p.tt("dve", a3[:, :, 64:128], v3(cq[hp]), pv3(0), ALU.mult)
                        p.cp("act", pct[:, hp * 8:hp * 8 + 8], bk(0).rr("p (c t) -> p c t", t=64)[:, :, 63])
                        bcast(1, beta)
                        p.tt("dve", kG[hp][:, 0:TT], ck[hp][:, 0:TT], bk(1), ALU.mult)
                        bcast(0, Ff)
                        p.tt("dve", bT[hp][:, 0:TT], ck[hp][:, 0:TT], bk(0), ALU.mult)
                        bcast(1, BF)
                        p.tt("dve", kT[hp][:, 0:TT], ck[hp][:, 0:TT], bk(1), ALU.mult)
                    pc = lambda hp, c: pct[:, hp * 8 + c: hp * 8 + c + 1]
                    self.dplr_tile(s, banks, arT, [t[:, 0:TT] for t in bT], [t[:, 0:TT] for t in kT],
                                   [t[:, 0:TT] for t in cv], pc, Hst, [t[:, 0:TT] for t in yo],
                                   arG=arG, bG=[t[:, 0:TT] for t in ck], kG=[t[:, 0:TT] for t in kG], gam=gam)
                    for hp in range(2):
                        y = yo[hp][:, 0:TT]
                        l2rn(y, tmp, tmp2, 0, 1e-6, b64)
                        p.stt("dve", y, y, col(P_GNW), tmp2[:, 0:TT], ALU.mult, ALU.mult)
                        p.tt("dve", yb16[hp][:], y, gt[hp][:, 0:TT], ALU.mult)
                        p.dma("pool", dv(self.y_d.ap()[256 + hp * 128:256 + (hp + 1) * 128, t0:t0 + TT], ("y", 2 + hp, j)), yb16[hp][:])
                if "attn" in self.mix_parts:
                    self.attn_seq(sq, W, banks, yb16, st)

    def attn_seq(self, sq, W, banks, yb16, st):
        p = self.p
        cs = self.cs
        pp = self.ppt
        bk = lambda i: dv(banks[i][:, :], f"pb{i}")
        bks = lambda i, sl: dv(banks[i][sl], f"pb{i}")
        zd = self.z_d.ap()
        ident = cs[:, C_ID:C_ID + 128]
        b64 = cs[:, C_B64:C_B64 + 128]
        if not hasattr(self, "_attn_tiles"):
            v1 = self.sb(st, [128, 5, 2, 65], F32, "v1")
            kt = self.sb(st, [128, 128 + TT], F32, "kt")
            osb = self.sb(st, [128, 512], F32, "osb")
            den = self.sb(st, [128, 8], F32, "den")
            ya = [self.sb(st, [128, TT], BF16, "ya") for _ in range(4)]
            self._attn_tiles = (v1, kt, osb, den, ya)
        v1, kt, osb, den, ya = self._attn_tiles
        for jt in range(self.mix_tiles):
            j = sq * TPS + jt
            t0 = j * TT
            first = (jt == 0)
            Q = W[0:4]
            tmp, tmp2 = W[4], W[5]
            E = W[6:10]
            p.memset("pool", v1[:, :, :, 64:65], 1.0)
            for t in range(4):
                for g in range(2):
                    h = g * 4 + t
                    r0 = 2056 + h * 64
                    gk = r0 // 128
                    gk2 = (r0 + 63) // 128
                    keys = set()
                    for gg in (gk, gk2):
                        keys.add(("z", gg - (gg % 2), j))
                        keys.add(("z", gg - (gg % 2) + 1, j))
                    p.dma("sp", Q[t][g * 64:g * 64 + 64, 0:TT], dv(zd[r0:r0 + 64, t0:t0 + TT], *keys))
            kkeys = [("z", 20, j), ("z", 21, j)]
            if first:
                p.memset("pool", kt[:, 0:128], 0.0)
                p.dma("sp", kt[:, 128:128 + TT], dv(zd[2568:2696, t0:t0 + TT], *kkeys))
            else:
                p.dma("sp", kt[:, 0:128 + TT], dv(zd[2568:2696, t0 - 128:t0 + TT], *(kkeys + [("z", 20, j - 1), ("z", 21, j - 1)])))
            vt = self.vtm_d.ap()
            if not first:
                p.dma("sp", v1[:, 0, :, 0:64], dv(vt[t0 - 128:t0, :].rearrange("p (g d) -> p g d", g=2), ("vtm", j - 1)))
            for q in range(4):
                p.dma("sp", v1[:, 1 + q, :, 0:64], dv(vt[t0 + q * 128:t0 + (q + 1) * 128, :].rearrange("p (g d) -> p g d", g=2), ("vtm", j)))
            for t in range(4):
                q = Q[t][:, 0:TT]
                p.act(tmp[:, 0:TT], q, AF.Square)
                p.mm(bk(0), b64, tmp[:, 0:TT])
                p.act(tmp2[:, 0:TT], bk(0), AF.Sqrt, bias=1e-6)
                p.recip(tmp2[:, 0:TT], tmp2[:, 0:TT])
                p.stt("dve", q, q, self.der[:, 0:1], tmp2[:, 0:TT], ALU.mult, ALU.mult)
            for part in range(2):
                if part == 0:
                    if first:
                        continue
                    ks = slice(0, 128)
                else:
                    ks = slice(128, 128 + TT)
                n = ks.stop - ks.start
                k = kt[:, ks]
                p.act(tmp[:, 0:n], k, AF.Square)
                p.mm(bks(1, (slice(0, 128), slice(0, n))), b64, tmp[:, 0:n])
                p.act(tmp2[:, 0:n], bks(1, (slice(0, 128), slice(0, n))), AF.Sqrt, bias=1e-6)
                p.recip(tmp2[:, 0:n], tmp2[:, 0:n])
                p.stt("dve", k, k, pp[:, P_KNW:P_KNW + 1], tmp2[:, 0:n], ALU.mult, ALU.mult)
            for n in range(4 if CUT > 1 else 0):
                qs = slice(n * 128, (n + 1) * 128)
                noprev = first and n == 0
                for g in range(2):
                    gs = slice(g * 64, g * 64 + 64)
                    for kind in range(2):
                        if kind == 0 and noprev:
                            continue
                        kcols = slice(n * 128 + kind * 128, n * 128 + kind * 128 + 128)
                        b = 2 + kind
                        for t in range(4):
                            p.mm(bks(b, (slice(0, 128), slice(t * 128, t * 128 + 128))), kt[gs, kcols], Q[t][gs, qs], inc=(t == 3))
                        Ek = E[kind * 2 + (g % 2)]
                        bo = (kind * 8 + g * 4) * 128
                        p.tt("dve", Ek[:, 0:TT], bk(b), self.bm[:, bo:bo + 512], ALU.add)
                        p.act(Ek[:, 0:TT], Ek[:, 0:TT], AF.Exp)
                    ob = 4 + g
                    if CUT <= 2:
                        continue
                    for t in range(4):
                        o = bks(ob, (slice(0, 128), slice(t * 65, t * 65 + 65)))
                        if not noprev:
                            p.mm(o, E[0 + g][:, t * 128:t * 128 + 128], v1[:, n, g, :], start=True, stop=False, inc=False)
                        p.mm(o, E[2 + g][:, t * 128:t * 128 + 128], v1[:, n + 1, g, :], start=noprev, stop=True, inc=(t == 3))
                    if CUT <= 3:
                        continue
                    o3 = bks(ob, (slice(0, 128), slice(0, 260))).rr("p (t e) -> p t e", e=65)
                    p.tt("dve", den[:, g * 4:g * 4 + 4], o3[:, :, 64], self.der[:, 2 + g * 4: 2 + g * 4 + 4], ALU.add)
                    p.recip(den[:, g * 4:g * 4 + 4], den[:, g * 4:g * 4 + 4])
                    for t in range(4):
                        h = g * 4 + t
                        if t % 2 == 0:
                            p.ts("dve", osb[:, h * 64:h * 64 + 64], o3[:, t, 0:64], den[:, h:h + 1], ALU.mult)
                        else:
                            p.act(osb[:, h * 64:h * 64 + 64], o3[:, t, 0:64], AF.Copy, scale=den[:, h:h + 1])
                TRV = int(os.environ.get("TRV", "0"))
                for hp4 in range(4 if CUT > 4 else 0):
                    tb = (6 if TRV != 1 else 0) + hp4 // 2
                    src_t = osb if TRV != 2 else Q[0]
                    p.tr(bks(tb, (slice(0, 128), slice((hp4 % 2) * 128, (hp4 % 2) * 128 + 128))),
                         src_t[:, hp4 * 128:(hp4 + 1) * 128], ident, inc=(hp4 % 2 == 1) or TRV == 3)
                for hh in range(2 if (CUT > 4 and CUT != 7) else 0):
                    for q2 in range(2):
                        hp4 = hh * 2 + q2
                        src = bks(6 + hh, (slice(0, 128), slice(q2 * 128, q2 * 128 + 128)))
                        if q2 == 0:
                            p.cp("act", ya[hp4][:, qs], src)
                        else:
                            p.cp("dve", ya[hp4][:, qs], src)
            for hp4 in range(4 if CUT not in (6, 7) else 0):
                p.dma("pool", dv(self.y_d.ap()[512 + hp4 * 128:512 + (hp4 + 1) * 128, t0:t0 + TT], ("y", 4 + hp4, j)), ya[hp4][:])

    def phase_outproj(self, l, xsrc, xdst):
        p = self.p
        if hasattr(self, "_attn_tiles"):
            del self._attn_tiles
        with contextlib.ExitStack() as st:
            wb = self.sb(st, [128, 8, D], BF16, "woutb")
            for c in range(8):
                p.dma("pool", wb[:, c, :], dv(self.w_out.ap()[l, c * 128:(c + 1) * 128, :], "w_out_d"))
            xts = [self.sb(st, [128, 8, TT], F32, "xt") for _ in range(2)]
            yts = [self.sb(st, [128, 8, TT], BF16, "yt") for _ in range(2)]
            banks = self.ps(st, 8)
            xs_v = xsrc.ap().rearrange("(c p) t -> p c t", p=128)
            xd_v = xdst.ap().rearrange("(c p) t -> p c t", p=128)
            yv = self.y_d.ap().rearrange("(c p) t -> p c t", p=128)
            for j in range(NTILE):
                t0 = j * TT
                xt, yt = xts[j % 2], yts[j % 2]
                for hh in range(2):
                    p.dma("sp", xt[:, hh * 4:(hh + 1) * 4, :], dv(xs_v[:, hh * 4:(hh + 1) * 4, t0:t0 + TT], ("x", id(xsrc), j)))
                p.dma("sp", yt[:], dv(yv[:, :, t0:t0 + TT], *[("y", c, j) for c in range(8)]))
                for m in range(8):
                    pv = dv(banks[m][:, :], f"pb{m}")
                    for c in range(8):
                        p.mm(pv, wb[:, c, m * 128:(m + 1) * 128], yt[:, c, :], start=(c == 0), stop=(c == 7), inc=(c == 7))
                    p.tt("dve", xt[:, m, :], xt[:, m, :], pv, ALU.add)
                for hh in range(2):
                    p.dma("pool", dv(xd_v[:, hh * 4:(hh + 1) * 4, t0:t0 + TT], ("x", id(xdst), j)), xt[:, hh * 4:(hh + 1) * 4, :])

    def phase_ffn(self, l, xsrc, xdst):
        p = self.p
        with contextlib.ExitStack() as st:
            w1 = self.sb(st, [128, 8, DFF], BF16, "w1b")
            w2 = self.sb(st, [128, 32, D], BF16, "w2b")
            for c in range(8):
                p.dma("pool", w1[:, c, :], dv(self.w_ff1.ap()[l, c * 128:(c + 1) * 128, :], "w1_d"))
            w2v = self.w_ff2.ap()[l].rearrange("(c p) n -> p c n", p=128)
            for c4 in range(8):
                p.dma("pool", w2[:, c4 * 4:(c4 + 1) * 4, :], dv(w2v[:, c4 * 4:(c4 + 1) * 4, :], "w2_d"))
            xt = self.sb(st, [128, 8, TT], F32, "xt")
            ht = self.sb(st, [128, 8, TT], BF16, "ht")
            h1 = self.sb(st, [128, 32, TT], BF16, "h1")
            sq2 = [self.sb(st, [128, TT], F32, "sq") for _ in range(2)]
            rstd = self.sb(st, [128, TT], F32, "rstd")
            banks = self.ps(st, 8)
            xs_v = xsrc.ap().rearrange("(c p) t -> p c t", p=128)
            xd_v = xdst.ap().rearrange("(c p) t -> p c t", p=128)
            for j in range(NTILE):
                t0 = j * TT
                for hh in range(2):
                    p.dma("sp", xt[:, hh * 4:(hh + 1) * 4, :], dv(xs_v[:, hh * 4:(hh + 1) * 4, t0:t0 + TT], ("x", id(xsrc), j)))
                self.rms_tile(xt, ht, P_LN2, sq2, rstd, banks[7], "pb7")
                for f in range(32):
                    b = f % 6
                    pv = dv(banks[b][:, :], f"pb{b}")
                    for c in range(8):
                        p.mm(pv, w1[:, c, f * 128:(f + 1) * 128], ht[:, c, :], start=(c == 0), stop=(c == 7), inc=(c == 7))
                    s = sq2[f % 2]
                    p.act(s[:], pv, AF.Relu)
                    p.tt("dve" if f % 2 == 0 else "pool", h1[:, f, :], s[:], s[:], ALU.mult)
                for m in range(8):
                    b = m % 6
                    pv = dv(banks[b][:, :], f"pb{b}")
                    for f in range(32):
                        p.mm(pv, w2[:, f, m * 128:(m + 1) * 128], h1[:, f, :], start=(f == 0), stop=(f == 31), inc=(f == 31))
                    p.tt("dve", xt[:, m, :], xt[:, m, :], pv, ALU.add)
                for hh in range(2):
                    p.dma("pool", dv(xd_v[:, hh * 4:(hh + 1) * 4, t0:t0 + TT], ("x", id(xdst), j)), xt[:, hh * 4:(hh + 1) * 4, :])


def _t5_bucket_np(dist):
    max_exact = 16
    nf = np.maximum(dist, max_exact).astype(np.float32)
    large = max_exact + (np.log(nf / max_exact) / np.float32(np.log(128 / max_exact)) * (32 - max_exact)).astype(np.int32)
    large = np.minimum(large, 31)
    return np.where(dist < max_exact, dist, large)


def _consts():
    c = np.zeros((128, NCST), np.float32)
    c[:, C_ID:C_ID + 128] = np.eye(128, dtype=np.float32)
    blk = np.kron(np.eye(2, dtype=np.float32), np.ones((64, 64), np.float32))
    c[:, C_B1:C_B1 + 128] = blk
    c[:, C_B64:C_B64 + 128] = blk / 64.0
    c[:, C_OD:C_OD + 128] = 1.0 / 1024.0
    rst = np.ones((128, TT), np.float32)
    rst[:, ::64] = 0.0
    c[:, C_RST:C_RST + TT] = rst
    s_ = np.arange(64)[:, None]
    t_ = np.arange(64)[None, :]
    strict = (s_ < t_).astype(np.float32)
    incl = (s_ <= t_).astype(np.float32)
    mt = np.concatenate([strict, incl, strict, incl], 1)
    c[0:64, C_MT4:C_MT4 + 1024] = np.tile(mt, (1, 4))
    msl = (t_ < s_).astype(np.float32)
    c[0:64, C_MSL4:C_MSL4 + 256] = np.tile(msl, (1, 4))
    c[0:64, C_I64:C_I64 + 256] = np.tile(np.eye(64, dtype=np.float32), (1, 4))
    for h in range(4):
        c[h, C_SEL + h * 64:C_SEL + (h + 1) * 64] = 1.0
    c[0:64, C_MNI:C_MNI + 64] = np.where(s_ <= t_, 0.0, NEG)
    jj = np.arange(128)[:, None]
    ii = np.arange(128)[None, :]
    c[:, C_BM:C_BM + 128] = np.where(jj > ii, 0.0, NEG)
    c[:, C_BM + 128:C_BM + 256] = np.where(jj <= ii, 0.0, NEG)
    return c


def _bias_band(rel_bias):
    i = np.arange(128)[None, :]
    j = np.arange(128)[:, None]
    out = np.zeros((128, 2, 8, 128), np.float32)
    d_prev = np.maximum(i + 128 - j, 0)
    d_cur = np.maximum(i - j, 0)
    for kind, dd in enumerate((d_prev, d_cur)):
        b = _t5_bucket_np(dd)
        out[:, kind, :, :] = np.transpose(rel_bias[b], (0, 2, 1))
    return out.reshape(128, 2 * 8 * 128)


def _pack_params(inp, l):
    pp = np.zeros((128, NPP), np.float32)
    f = lambda a, n: np.asarray(a, np.float32).reshape(n, 128).T
    pp[:, P_LN1:P_LN1 + 8] = f(inp["ln1_w"][l], 8)
    pp[:, P_LN2:P_LN2 + 8] = f(inp["ln2_w"][l], 8)
    pp[:, P_MU:P_MU + 8] = f(inp["rwkv_mu"][l], 8)
    pp[:, P_W0:P_W0 + 2] = f(inp["rwkv_w0"][l], 2)
    pp[:, P_A0:P_A0 + 2] = f(inp["rwkv_a0"][l], 2)
    pp[:, P_KK:P_KK + 2] = f(inp["rwkv_k_k"][l], 2)
    pp[:, P_KA:P_KA + 2] = f(inp["rwkv_k_a"][l], 2)
    pp[:, P_RK:P_RK + 2] = f(inp["rwkv_r_k"][l].reshape(256), 2)
    pp[:, P_LXW:P_LXW + 2] = f(inp["rwkv_lnx_w"][l], 2)
    pp[:, P_LXB:P_LXB + 2] = f(inp["rwkv_lnx_b"][l], 2)
    cw = np.asarray(inp["gdn_conv_w"][l], np.float32)
    for i in range(6):
        pp[:, P_CONV + i * 4:P_CONV + i * 4 + 4] = cw[:, i * 128:(i + 1) * 128].T
    pp[:, P_GNW] = np.tile(inp["gdn_norm_w"][l], 2)
    pp[:, P_QNW] = np.tile(inp["attn_q_norm_w"][l], 2)
    pp[:, P_KNW] = np.tile(inp["attn_k_norm_w"][l], 2)
    pp[0:4, P_ALOG] = inp["gdn_a_log"][l]
    pp[0:4, P_DTB] = inp["gdn_dt_bias"][l]
    pp[:, P_SINK:P_SINK + 8] = np.asarray(inp["attn_sinks"][l], np.float32)[None, :]
    return pp


_NC_CACHE = {}
LAYERS_PER_LAUNCH = 4


def _get_nc(nl):
    if nl not in _NC_CACHE:
        _NC_CACHE[nl] = Builder(nl).build()
    return _NC_CACHE[nl]


def kernel(**inp):
    inp = {k: np.asarray(v) for k, v in inp.items()}
    x = inp["x"].astype(np.float32)
    depth = inp["w_in"].shape[0]
    cst = _consts()
    bband = _bias_band(inp["rel_bias"].astype(np.float32))
    xT = [np.ascontiguousarray(x[2 * c:2 * c + 2].reshape(NTOK, D).T) for c in range(NCORES)]
    nl = LAYERS_PER_LAUNCH
    for l0 in range(0, depth, nl):
        nc = _get_nc(nl)
        sl = slice(l0, l0 + nl)
        pp = np.stack([_pack_params(inp, l) for l in range(l0, l0 + nl)])
        shared = {
            "w_in": np.ascontiguousarray(inp["w_in"][sl], np.float32),
            "w_out": np.ascontiguousarray(inp["w_out"][sl], np.float32),
            "w_ff1": np.ascontiguousarray(inp["w_ff1"][sl], np.float32),
            "w_ff2": np.ascontiguousarray(inp["w_ff2"][sl], np.float32),
            "w_up": np.ascontiguousarray(inp["rwkv_w_up"][sl], np.float32),
            "a_up": np.ascontiguousarray(inp["rwkv_a_up"][sl], np.float32),
            "g_up": np.ascontiguousarray(inp["rwkv_g_up"][sl], np.float32),
            "pp": pp, "cst": cst, "bband": bband,
        }
        in_maps = [dict(shared, xT=xT[c]) for c in range(NCORES)]
        res = run_bass_kernel_spmd(nc, in_maps, core_ids=list(range(NCORES)))
        xT = [np.ascontiguousarray(res.results[c]["outT"]) for c in range(NCORES)]
    out = np.stack([xT[c].T.reshape(NSEQ, SEQ, D) for c in range(NCORES)]).reshape(16, SEQ, D)
    return out.astype(np.float32)
```

```python
import contextlib
import os
CUT = int(os.environ.get('CUT', '99'))
ALLINC = int(os.environ.get('ALLINC', '0'))
ROWSER = int(os.environ.get('ROWSER', '1'))
import numpy as np
import concourse.bass as bass
import concourse.mybir as mybir
from concourse.bass_utils import run_bass_kernel_spmd

F32 = mybir.dt.float32
BF16 = mybir.dt.bfloat16
AF = mybir.ActivationFunctionType
ALU = mybir.AluOpType

NCORES = 8
SEQ = 2048
NSEQ = 2
NTOK = NSEQ * SEQ
D = 1024
DIN = 2824
DFF = 4096
TT = 512
NTILE = NTOK // TT
TPS = SEQ // TT
CH = 64
NCH = TT // CH
NEG = -30000.0
DECAY_C = -0.6065306597126334

C_ID = 0
C_B1 = 128
C_B64 = 256
C_OD = 384
C_RST = 512
C_MT4 = 1024
C_MSL4 = 2048
C_I64 = 2304
C_SEL = 2560
C_BM = 2816
C_MNI = 3072
NCST = 3136
P_LN1, P_LN2, P_MU, P_W0, P_A0, P_KK, P_KA, P_RK, P_LXW, P_LXB, P_CONV = 0, 8, 16, 24, 26, 28, 30, 32, 34, 36, 38
P_GNW, P_QNW, P_KNW, P_ALOG, P_DTB, P_SINK = 62, 63, 64, 65, 66, 67
NPP = 75


class Buf:
    __slots__ = ("name", "w", "r", "pe_rows")

    def __init__(self, name):
        self.name = name
        self.w = None
        self.r = {}
        self.pe_rows = None


class View:
    __slots__ = ("ap", "keys")

    def __init__(self, ap, keys):
        self.ap = ap
        self.keys = keys

    def __getitem__(self, idx):
        return View(self.ap[idx], self.keys)

    def rr(self, pat, **kw):
        return View(self.ap.rearrange(pat, **kw), self.keys)


class Tl:
    def __init__(self, h, key):
        self.h = h
        self.key = key

    def __getitem__(self, idx):
        return View(self.h[idx], (self.key,))


def dv(ap, *keys):
    return View(ap, tuple(keys))


class Prog:
    COMPUTE = ("pe", "act", "dve", "pool")

    def __init__(self, nc, ndma=14):
        self.nc = nc
        self.E = {"pe": nc.tensor, "act": nc.scalar, "dve": nc.vector, "pool": nc.gpsimd, "sp": nc.sync}
        self.sem = {e: nc.alloc_semaphore("c_" + e) for e in self.COMPUTE}
        self.cnt = {e: 0 for e in self.COMPUTE}
        self.known = {e: {} for e in self.E}
        self.dsem = {}
        for q in ("sp", "pool"):
            self.dsem[q] = [[nc.alloc_semaphore(f"d_{q}{i}"), 0] for i in range(ndma)]
        self.drr = {q: 0 for q in self.dsem}
        self.bufs = {}
        self.nins = 0
        self.log = {e: [] for e in self.E}

    def buf(self, key):
        b = self.bufs.get(key)
        if b is None:
            b = self.bufs[key] = Buf(key)
        return b

    def _need(self, eng, ev, waits):
        if ev is None:
            return
        sem, val = ev
        if self.known[eng].get(sem, 0) >= val:
            return
        if val > waits.get(sem, 0):
            waits[sem] = val

    def _deps(self, eng, reads, writes, is_dma):
        waits = {}
        mysem = None if is_dma else self.sem[eng]
        for b in reads:
            self._need(eng, b.w, waits)
        for b in writes:
            if b.w is not None and (is_dma or b.w[0] is not mysem):
                self._need(eng, b.w, waits)
            for s, v in b.r.items():
                if is_dma or s is not mysem:
                    self._need(eng, (s, v), waits)
        return waits

    def _commit(self, ev, reads, writes):
        s, v = ev
        for b in reads:
            if b.r.get(s, 0) < v:
                b.r[s] = v
        for b in writes:
            b.w = ev
            b.r = {}

    def _bl(self, views):
        out = []
        for v in views:
            if v is None or not isinstance(v, View):
                continue
            for k in v.keys:
                out.append(self.buf(k))
        return out

    def op(self, eng, fn, reads=(), writes=(), inc=True, extra=()):
        if ALLINC or eng == "pe":
            inc = True
        reads = self._bl(reads)
        writes = self._bl(writes)
        for b in reads:
            if isinstance(b.name, str) and b.name.startswith("pb") and b not in writes:
                writes.append(b)
        waits = self._deps(eng, reads, writes, False)
        for ev in extra:
            self._need(eng, ev, waits)
        e = self.E[eng]
        for s, v in waits.items():
            self.known[eng][s] = v
            e.wait_ge(s, v)
            self.nins += 1
        sem = self.sem[eng]
        ins = fn(e)
        self.nins += 1
        self.log[eng].append((list(waits.items()), (sem, 1) if inc else None))
        if inc:
            self.cnt[eng] += 1
            ev = (sem, self.cnt[eng])
            ins.then_inc(sem, 1)
        else:
            ev = (sem, self.cnt[eng] + 1)
        self._commit(ev, reads, writes)

    def dma(self, q, out, in_):
        reads = self._bl([in_])
        writes = self._bl([out])
        waits = self._deps(q, reads, writes, True)
        pool = self.dsem[q]
        i = self.drr[q]
        self.drr[q] = (i + 1) % len(pool)
        sem, n = pool[i]
        if n > 0:
            self._need(q, (sem, 16 * n), waits)
        pool[i][1] = n + 1
        ev = (sem, 16 * (n + 1))
        e = self.E[q]
        for s, v in waits.items():
            self.known[q][s] = v
            e.wait_ge(s, v)
            self.nins += 1
        e.dma_start(out=out.ap, in_=in_.ap).then_inc(sem, 16)
        self.nins += 1
        self.log[q].append((list(waits.items()), (sem, 16)))
        self._commit(ev, reads, writes)

    def simulate(self):
        pos = {e: 0 for e in self.log}
        val = {}
        prog = True
        while prog:
            prog = False
            for e, lst in self.log.items():
                while pos[e] < len(lst):
                    waits, inc = lst[pos[e]]
                    if all(val.get(s_, 0) >= v for s_, v in waits):
                        if inc is not None:
                            val[inc[0]] = val.get(inc[0], 0) + inc[1]
                        pos[e] += 1
                        prog = True
                    else:
                        break
        stuck = {e: (pos[e], len(l)) for e, l in self.log.items() if pos[e] < len(l)}
        for e in stuck:
            waits, inc = self.log[e][pos[e]]
            print("STUCK", e, pos[e], [(s_.name, v, val.get(s_, 0)) for s_, v in waits])
        return stuck

    def barrier(self):
        targets = [(self.sem[e], self.cnt[e]) for e in self.COMPUTE if self.cnt[e] > 0]
        for q in self.dsem:
            targets += [(s_, 16 * n) for s_, n in self.dsem[q] if n > 0]
        for eng, e in self.E.items():
            wl = []
            for s_, v in targets:
                if self.known[eng].get(s_, 0) < v:
                    self.known[eng][s_] = v
                    e.wait_ge(s_, v)
                    wl.append((s_, v))
                    self.nins += 1
            self.log[eng].append((wl, None))

    def wait_all_dma(self, q):
        e = self.E[q]
        for s, n in self.dsem[q]:
            if n > 0:
                e.wait_ge(s, 16 * n)

    def _pe_rows(self, out, lhsT):
        b0 = lhsT.ap.base_partition()
        k = lhsT.ap.partition_size()
        rows = (b0 // 32, (b0 + k + 31) // 32)
        extra = []
        if ROWSER:
            lr = getattr(self, "_last_rows", None)
            if lr is not None and (lr[1] <= rows[0] or rows[1] <= lr[0]) and self.cnt["pe"] > 0:
                extra.append((self.sem["pe"], self.cnt["pe"]))
            self._last_rows = rows
        for key in out.keys:
            b = self.buf(key)
            pr = b.pe_rows
            if pr is not None and (pr[1] <= rows[0] or rows[1] <= pr[0]) and b.w is not None and b.w[0] is self.sem["pe"]:
                extra.append(b.w)
            b.pe_rows = rows
        return extra

    def mm(self, out, lhsT, rhs, start=True, stop=True, inc=True):
        extra = self._pe_rows(out, lhsT)
        self.op("pe", lambda e: e.matmul(out.ap, lhsT.ap, rhs.ap, start=start, stop=stop),
                reads=[lhsT, rhs], writes=[out], inc=inc, extra=extra)

    def tr(self, out, in_, ident, inc=True):
        extra = self._pe_rows(out, in_)
        if os.environ.get("TRMODE", "tr") == "mm":
            self.op("pe", lambda e: e.matmul(out.ap, in_.ap, ident.ap, start=True, stop=True), reads=[in_, ident], writes=[out], extra=extra)
        else:
            self.op("pe", lambda e: e.transpose(out.ap, in_.ap, ident.ap), reads=[in_, ident], writes=[out], extra=extra)

    def act(self, out, in_, func, bias=None, scale=None, eng="act"):
        kw = {}
        rd = [in_]
        if bias is not None:
            if isinstance(bias, View):
                kw["bias"] = bias.ap
                rd.append(bias)
            else:
                kw["bias"] = bias
        if scale is not None:
            if isinstance(scale, View):
                kw["scale"] = scale.ap
                rd.append(scale)
            else:
                kw["scale"] = scale
        self.op("act", lambda e: e.activation(out.ap, in_.ap, func, **kw), reads=rd, writes=[out])

    def tt(self, eng, out, a, b, op):
        self.op(eng, lambda e: e.tensor_tensor(out.ap, a.ap, b.ap, op), reads=[a, b], writes=[out])

    def ts(self, eng, out, a, s1, op0, s2=None, op1=None):
        rd = [a]
        v1 = s1
        if isinstance(s1, View):
            rd.append(s1)
            v1 = s1.ap
        v2 = s2
        if isinstance(s2, View):
            rd.append(s2)
            v2 = s2.ap
        if op1 is None:
            self.op(eng, lambda e: e.tensor_scalar(out.ap, a.ap, v1, None, op0), reads=rd, writes=[out])
        else:
            self.op(eng, lambda e: e.tensor_scalar(out.ap, a.ap, v1, v2, op0, op1), reads=rd, writes=[out])

    def stt(self, eng, out, a, s, b, op0, op1):
        eng = "dve"
        rd = [a, b]
        sv = s
        if isinstance(s, View):
            rd.append(s)
            sv = s.ap
        self.op(eng, lambda e: e.scalar_tensor_tensor(out.ap, a.ap, sv, b.ap, op0, op1), reads=rd, writes=[out])

    def cp(self, eng, out, in_):
        if eng == "act":
            self.op("act", lambda e: e.copy(out.ap, in_.ap), reads=[in_], writes=[out])
        else:
            self.op(eng, lambda e: e.tensor_copy(out.ap, in_.ap), reads=[in_], writes=[out])

    def memset(self, eng, out, val):
        self.op(eng, lambda e: e.memset(out.ap, val), writes=[out])

    def recip(self, out, in_):
        self.op("dve", lambda e: e.reciprocal(out.ap, in_.ap), reads=[in_], writes=[out])

    def scan(self, out, d0, d1):
        self.op("dve", lambda e: e.tensor_tensor_scan(out.ap, d0.ap, d1.ap, 0.0, ALU.mult, ALU.add),
                reads=[d0, d1], writes=[out])


class Builder:
    def __init__(self, nlayers, debug=False, stop_after=None):
        self.stop_after = stop_after
        self.mix_parts = ("rwkv", "gdn", "attn")
        self.mix_tiles = TPS
        self.mix_nseq = NSEQ
        if stop_after and stop_after.startswith("mix:"):
            f = stop_after.split(":")
            self.mix_parts = tuple(f[1].split(","))
            self.mix_tiles = int(f[2])
            self.mix_nseq = int(f[3])
        self.L = nlayers
        nc = self.nc = bass.Bass("TRN2", target_bir_lowering=False)
        self.p = Prog(nc)
        L = nlayers
        di = lambda n, s, dt=F32: nc.dram_tensor(n, s, dt, kind="ExternalInput")
        self.xT = di("xT", [D, NTOK])
        self.w_in = di("w_in", [L, D, DIN])
        self.w_out = di("w_out", [L, D, D])
        self.w_ff1 = di("w_ff1", [L, D, DFF])
        self.w_ff2 = di("w_ff2", [L, DFF, D])
        self.w_up = di("w_up", [L, 64, 256])
        self.a_up = di("a_up", [L, 64, 256])
        self.g_up = di("g_up", [L, 128, 256])
        self.pp = di("pp", [L, 128, NPP])
        self.cst = di("cst", [128, NCST])
        self.bband = di("bband", [128, 2 * 8 * 128])
        self.outT = nc.dram_tensor("outT", [D, NTOK], F32, kind="ExternalOutput")
        self.xa = nc.dram_tensor("xa_d", [D, NTOK], F32)
        kd = "ExternalOutput" if debug else "Internal"
        self.xb = nc.dram_tensor("xb_d", [D, NTOK], F32, kind=kd)
        self.z_d = nc.dram_tensor("z_d", [DIN, NTOK], F32, kind=kd)
        self.vtm_d = nc.dram_tensor("vtm_d", [NTOK, 128], F32, kind=kd)
        self.y_d = nc.dram_tensor("y_d", [D, NTOK], BF16, kind=kd)
        self._uid = 0

    def sb(self, st, shape, dt=F32, name=None):
        self._uid += 1
        name = name or f"t{self._uid}"
        h = st.enter_context(self.nc.sbuf_tensor(f"{name}_{self._uid}", shape, dt))
        return Tl(h, f"{name}_{self._uid}")

    def ps(self, st, n):
        out = []
        for i in range(n):
            self._uid += 1
            h = st.enter_context(self.nc.psum_tensor(f"ps{self._uid}", [128, 512], F32))
            out.append(h)
        return out

    def build(self):
        p = self.p
        with contextlib.ExitStack() as st:
            self.cs = self.sb(st, [128, NCST], F32, "cst")
            p.dma("sp", self.cs[:], dv(self.cst.ap(), "cst_d"))
            self.ppt = self.sb(st, [128, NPP], F32, "pp")
            self.der = self.sb(st, [128, 12], F32, "der")
            cur = self.xT
            for l in range(self.L):
                p.dma("sp", self.ppt[:], dv(self.pp.ap()[l], "pp_d"))
                p.ts("dve", self.der[:, 0:1], self.ppt[:, P_QNW:P_QNW + 1], 0.125, ALU.mult)
                p.act(self.der[0:4, 1:2], self.ppt[0:4, P_ALOG:P_ALOG + 1], AF.Exp)
                p.ts("dve", self.der[0:4, 1:2], self.der[0:4, 1:2], -1.0, ALU.mult)
                p.act(self.der[:, 2:10], self.ppt[:, P_SINK:P_SINK + 8], AF.Exp)
                last = (l == self.L - 1)
                self.phase_inproj(l, cur)
                p.barrier()
                if self.stop_after == "inproj":
                    break
                self.phase_mix(l)
                p.barrier()
                if self.stop_after and self.stop_after.startswith("mix"):
                    break
                self.phase_outproj(l, cur, self.xb)
                p.barrier()
                if self.stop_after == "outproj":
                    break
                dst = self.outT if last else self.xa
                self.phase_ffn(l, self.xb, dst)
                p.barrier()
                cur = self.xa
            p.wait_all_dma("pool")
            p.wait_all_dma("sp")
        return self.nc

    def rms_tile(self, xt, ht, lncol, sq2, rstd, pbank, pkey):
        p = self.p
        ones = self.cs[:, C_OD:C_OD + 128]
        pv = dv(pbank[:, :], pkey)
        for c in range(8):
            s = sq2[c % 2]
            p.act(s[:], xt[:, c, :], AF.Square)
            p.mm(pv, ones, s[:], start=(c == 0), stop=(c == 7), inc=(c == 7) or True)
        p.act(rstd[:], pv, AF.Sqrt, bias=1e-6)
        p.recip(rstd[:], rstd[:])
        for c in range(8):
            eng = "dve" if c % 2 == 0 else "pool"
            p.stt(eng, ht[:, c, :], xt[:, c, :], self.ppt[:, lncol + c: lncol + c + 1], rstd[:], ALU.mult, ALU.mult)

    def phase_inproj(self, l, xsrc):
        p = self.p
        with contextlib.ExitStack() as st:
            wb = self.sb(st, [128, 8, DIN], BF16, "winb")
            for c in range(8):
                p.dma("pool", wb[:, c, :], dv(self.w_in.ap()[l, c * 128:(c + 1) * 128, :], "w_in_d"))
            xts = [self.sb(st, [128, 8, TT], F32, "xt") for _ in range(2)]
            ht = self.sb(st, [128, 8, TT], BF16, "ht")
            sq2 = [self.sb(st, [128, TT], F32, "sq") for _ in range(2)]
            rstd = self.sb(st, [128, TT], F32, "rstd")
            stg = [self.sb(st, [128, 2, TT], F32, "stg") for _ in range(3)]
            vst = self.sb(st, [128, 4, 128], F32, "vst")
            banks = self.ps(st, 8)
            xsrc_v = xsrc.ap().rearrange("(c p) t -> p c t", p=128)
            ngrp = 22
            for j in range(NTILE):
                xt = xts[j % 2]
                t0 = j * TT
                for hh in range(2):
                    p.dma("sp", xt[:, hh * 4:(hh + 1) * 4, :], dv(xsrc_v[:, hh * 4:(hh + 1) * 4, t0:t0 + TT], ("x", id(xsrc), j)))
                self.rms_tile(xt, ht, P_LN1, sq2, rstd, banks[7], "pb7")
                gi = 0
                for g in range(ngrp):
                    bk = banks[g % 6]
                    pv = dv(bk[:, :], f"pb{g % 6}")
                    for c in range(8):
                        p.mm(pv, wb[:, c, g * 128:(g + 1) * 128], ht[:, c, :], start=(c == 0), stop=(c == 7), inc=(c == 7))
                    s = stg[(g // 2) % 3]
                    if g % 2 == 0:
                        p.cp("act", s[:, 0, :], pv)
                    else:
                        p.cp("dve", s[:, 1, :], pv)
                    if g % 2 == 1 or g == ngrp - 1:
                        g0 = g - (g % 2)
                        ng = g - g0 + 1
                        dst = self.z_d.ap()[g0 * 128:(g0 + ng) * 128, t0:t0 + TT].rearrange("(g p) t -> p g t", p=128)
                        p.dma("pool", dv(dst, ("z", g0, j), ("z", g0 + 1, j)), s[:, 0:ng, :])
                pv = dv(banks[6][:, :], "pb6")
                for q in range(4):
                    for c in range(8):
                        p.mm(dv(banks[6][:, q * 128:(q + 1) * 128], "pb6"), ht[:, c, q * 128:(q + 1) * 128],
                             wb[:, c, 2696:2824], start=(c == 0), stop=(c == 7), inc=(c == 7))
                p.cp("act", vst[:].rr("p a b -> p (a b)"), pv)
                dst = self.vtm_d.ap()[t0:t0 + TT, :].rearrange("(q p) f -> p q f", p=128)
                p.dma("pool", dv(dst, ("vtm", j)), vst[:])

    def dplr_setup(self, st):
        s = {}
        s["tmV"] = self.sb(st, [64, NCH, 256], F32, "tmV")
        s["tmB"] = self.sb(st, [64, NCH, 256], F32, "tmB")
        s["tmK"] = self.sb(st, [64, NCH, 256], F32, "tmK")
        s["G3"] = self.sb(st, [64, NCH, 4, 192], F32, "G3")
        s["Mb"] = [self.sb(st, [64, 4, 2, 256], F32, "Mb") for _ in range(2)]
        s["Tt"] = self.sb(st, [64, NCH, 256], F32, "Tt")
        s["DT"] = self.sb(st, [64, 256], F32, "DT")
        s["DTm"] = self.sb(st, [64, 1024], F32, "DTm")
        s["gcol"] = self.sb(st, [64, 32], F32, "gcol")
        s["Hs"] = [self.sb(st, [128, 64], F32, "Hs") for _ in range(2)]
        s["Xs"] = self.sb(st, [64, 256], F32, "Xs")
        s["Us"] = self.sb(st, [64, 256], F32, "Us")
        return s

    def dplr_tile(self, s, banks, arT, bT, kT, vT, pc, H, yout, arG=None, bG=None, kG=None, gam=None):
        p = self.p
        cs = self.cs
        ident = cs[:, C_ID:C_ID + 128]
        bk = lambda i: dv(banks[i][:, :], f"pb{i}")
        bks = lambda i, sl: dv(banks[i][sl], f"pb{i}")
        R64 = slice(0, 64)
        scalar_mode = gam is not None
        if arG is None:
            arG, bG, kG = arT, bT, kT
        slot_of = lambda h: (h % 2) * 2 + h // 2
        if scalar_mode:
            for c in range(NCH):
                p.tr(bks(7, (R64, slice(c * 4, c * 4 + 4))), gam[0:4, c * 64:(c + 1) * 64], cs[0:4, C_ID:C_ID + 4])
            p.cp("act", s["gcol"][:], bks(7, (R64, slice(0, 32))))
        for ti, (src, dst) in enumerate(((vT, s["tmV"]), (bT, s["tmB"]), (kT, s["tmK"]))):
            for cp2 in range(NCH // 2):
                b = 2 + (ti * (NCH // 2) + cp2) % 2
                for cc in range(2):
                    c = cp2 * 2 + cc
                    for hp in range(2):
                        p.tr(bks(b, (R64, slice(cc * 256 + hp * 128, cc * 256 + hp * 128 + 128))),
                             src[hp][:, c * 64:(c + 1) * 64], ident)
                eng = "act" if (cp2 % 2 == 0) else "dve"
                p.cp(eng, dst[:, cp2 * 2:cp2 * 2 + 2, :].rr("p a b -> p (a b)"), bks(b, (R64, slice(0, 512))))
        G3, Tt = s["G3"], s["Tt"]
        for cb in range(NCH // 4):
            Mb0 = s["Mb"][0]
            for ci in range(4):
                c = cb * 4 + ci
                csl = slice(c * 64, (c + 1) * 64)
                for h in range(4):
                    hp, sl = h // 2, slice((h % 2) * 64, (h % 2) * 64 + 64)
                    gb = 4 + (h % 2)
                    o = (h // 2) * 256
                    p.mm(bks(gb, (R64, slice(o, o + 128))), bG[hp][sl, csl], arG[hp][sl, c, :])
                    p.mm(bks(gb, (R64, slice(o + 128, o + 256))), kG[hp][sl, csl], arG[hp][sl, c, :])
                if scalar_mode:
                    for h in range(4):
                        so = slot_of(h) * 64
                        p.mm(bks(6, (R64, slice(so, so + 64))), cs[0:4, C_SEL + h * 64:C_SEL + h * 64 + 64], gam[0:4, csl])
                    for h in range(4):
                        so = slot_of(h) * 64
                        p.stt("dve", s["DT"][:, so:so + 64], bks(6, (R64, slice(so, so + 64))),
                              s["gcol"][:, c * 4 + h:c * 4 + h + 1], cs[0:64, C_MNI:C_MNI + 64], ALU.subtract, ALU.add)
                    p.act(s["DT"][:], s["DT"][:], AF.Exp)
                    dt4 = View(s["DT"][:].ap.rearrange("p (h t) -> p h t", h=4).unsqueeze(2).broadcast_to([64, 4, 4, 64]), s["DT"][:].keys)
                    p.tt("dve", s["DTm"][:].rr("p (h b t) -> p h b t", h=4, b=4), cs[0:64, C_MT4:C_MT4 + 1024].rr("p (h b t) -> p h b t", h=4, b=4),
                         dt4, ALU.mult)
                    mk = lambda par: s["DTm"][:, par * 512:par * 512 + 512]
                else:
                    mk = lambda par: cs[0:64, C_MT4 + par * 512: C_MT4 + par * 512 + 512]
                for par in range(2):
                    Pv = bks(4 + par, (R64, slice(0, 512))).rr("p (a b) -> p a b", a=2)
                    Mv = mk(par).rr("p (a b) -> p a b", a=2)
                    p.tt("dve", G3[:, c, par * 2:par * 2 + 2, :], Pv[:, :, 64:256], Mv[:, :, 64:256], ALU.mult)
                    p.tt("dve", Mb0[:, ci, 1, par * 128:(par + 1) * 128].rr("p (a t) -> p a t", a=2), Pv[:, :, 0:64], Mv[:, :, 0:64], ALU.mult)
                for sl_ in range(4):
                    ms = slice(sl_ * 64, sl_ * 64 + 64)
                    p.tr(bks(6, (R64, slice(256 + sl_ * 64, 256 + sl_ * 64 + 64))), Mb0[:, ci, 1, ms], cs[0:64, C_ID:C_ID + 64])
                p.cp("act", Mb0[:, ci, 0, :], bks(6, (R64, slice(256, 512))))
                p.tt("pool", Tt[:, c, :], Mb0[:, ci, 1, :], cs[0:64, C_I64:C_I64 + 256], ALU.add)
            cur = 0
            for lvl in range(5):
                Mc = s["Mb"][cur]
                Mn = s["Mb"][1 - cur]
                lastl = (lvl == 4)
                for ci in range(4):
                    for sl_ in range(4):
                        ms = slice(sl_ * 64, sl_ * 64 + 64)
                        p.mm(bks(ci, (R64, ms)), Mc[:, ci, 1, ms], Mc[:, ci, 0, ms])
                    if not lastl:
                        for sl_ in range(4):
                            ms = slice(sl_ * 64, sl_ * 64 + 64)
                            p.mm(bks(ci, (R64, slice(256 + sl_ * 64, 256 + sl_ * 64 + 64))), Mc[:, ci, 0, ms], Mc[:, ci, 1, ms])
                for ci in range(4):
                    eng = "act" if ci % 2 == 0 else "dve"
                    if not lastl:
                        p.cp(eng, Mn[:, ci, :, :].rr("p a b -> p (a b)"), bks(ci, (R64, slice(0, 512))))
                    else:
                        p.cp(eng, Mn[:, ci, 0, :], bks(ci, (R64, slice(0, 256))))
                for ci in range(4):
                    c = cb * 4 + ci
                    pb, po = 6 + ci // 2, (ci % 2) * 256
                    for sl_ in range(4):
                        ms = slice(sl_ * 64, sl_ * 64 + 64)
                        p.mm(bks(pb, (R64, slice(po + sl_ * 64, po + sl_ * 64 + 64))), Mn[:, ci, 0, ms], Tt[:, c, ms])
                for ci in range(4):
                    c = cb * 4 + ci
                    pb, po = 6 + ci // 2, (ci % 2) * 256
                    p.tt("dve", Tt[:, c, :], Tt[:, c, :], bks(pb, (R64, slice(po, po + 256))), ALU.add)
                cur = 1 - cur
        Xs, Us = s["Xs"], s["Us"]
        for c in range(NCH):
            csl = slice(c * 64, (c + 1) * 64)
            for h in range(4):
                hp, sl = h // 2, slice((h % 2) * 64, (h % 2) * 64 + 64)
                hs = slice(h * 64, h * 64 + 64)
                so = slot_of(h)
                o = bks(0, (R64, hs))
                p.mm(o, arT[hp][sl, c, 0:64], H[hp][sl, :], start=True, stop=False)
                p.mm(o, G3[:, c, so, 64:128], s["tmV"][:, c, hs], start=False, stop=True)
            p.cp("act", Xs[:], bks(0, (R64, slice(0, 256))))
            for h in range(4):
                hs = slice(h * 64, h * 64 + 64)
                ms = slice(slot_of(h) * 64, slot_of(h) * 64 + 64)
                p.mm(bks(0, (R64, slice(256 + h * 64, 256 + h * 64 + 64))), Tt[:, c, ms], Xs[:, hs])
            p.cp("dve", Us[:], bks(0, (R64, slice(256, 512))))
            for h in range(4):
                hp, sl = h // 2, slice((h % 2) * 64, (h % 2) * 64 + 64)
                hs = slice(h * 64, h * 64 + 64)
                so = slot_of(h)
                yb = 1 if hp == 0 else 3
                o = bks(yb, (sl, csl))
                p.mm(o, H[hp][sl, :], arT[hp][sl, c, 64:128], start=True, stop=False)
                p.mm(o, Us[:, hs], G3[:, c, so, 0:64], start=False, stop=False)
                p.mm(o, s["tmV"][:, c, hs], G3[:, c, so, 128:192], start=False, stop=True)
            if scalar_mode:
                for hp in range(2):
                    p.act(s["Hs"][hp][:], H[hp][:], AF.Copy, scale=pc(hp, c))
                Hsrc = s["Hs"]
            else:
                Hsrc = H
            for h in range(4):
                hp, sl = h // 2, slice((h % 2) * 64, (h % 2) * 64 + 64)
                hs = slice(h * 64, h * 64 + 64)
                o = bks(2, (sl, slice(hp * 64, hp * 64 + 64)))
                p.mm(o, cs[sl, C_ID + (h % 2) * 64: C_ID + (h % 2) * 64 + 64], Hsrc[hp][sl, :], start=True, stop=False)
                p.mm(o, s["tmB"][:, c, hs], Us[:, hs], start=False, stop=False)
                p.mm(o, s["tmK"][:, c, hs], s["tmV"][:, c, hs], start=False, stop=True)
            for hp in range(2):
                if scalar_mode:
                    p.cp("act", H[hp][:], bks(2, (slice(0, 128), slice(hp * 64, hp * 64 + 64))))
                else:
                    p.act(H[hp][:], bks(2, (slice(0, 128), slice(hp * 64, hp * 64 + 64))), AF.Copy, scale=pc(hp, c))
        for hp in range(2):
            yb = 1 if hp == 0 else 3
            p.cp("act" if hp == 0 else "dve", yout[hp][:], bk(yb))

    def phase_mix(self, l):
        p = self.p
        cs = self.cs
        pp = self.ppt
        with contextlib.ExitStack() as st:
            banks = self.ps(st, 8)
            bk = lambda i: dv(banks[i][:, :], f"pb{i}")
            bks = lambda i, sl: dv(banks[i][sl], f"pb{i}")
            self.bm = self.sb(st, [128, 2 * 8 * 128], F32, "biasm")
            p.dma("sp", self.bm[:], dv(self.bband.ap(), "bb_d"))
            for kind in range(2):
                for h in range(8):
                    o = self.bm[:, (kind * 8 + h) * 128:(kind * 8 + h + 1) * 128]
                    p.tt("dve", o, o, self.cs[:, C_BM + kind * 128: C_BM + (kind + 1) * 128], ALU.add)
            s = self.dplr_setup(st)
            W = [self.sb(st, [128, TT + 3], F32, "W") for _ in range(26)]
            arT = [self.sb(st, [128, NCH, 128], F32, "arT") for _ in range(2)]
            arG = [self.sb(st, [128, NCH, 128], F32, "arG") for _ in range(2)]
            Hst = [self.sb(st, [128, 64], F32, "H") for _ in range(2)]
            sm = [self.sb(st, [4, TT], F32, "sm") for _ in range(7)]
            yb16 = [self.sb(st, [128, TT], BF16, "yb") for _ in range(2)]
            wup = self.sb(st, [128, 256], F32, "wup")
            aup = self.sb(st, [128, 256], F32, "aup")
            gup = self.sb(st, [128, 256], F32, "gup")
            p.dma("sp", wup[0:64, :], dv(self.w_up.ap()[l], "wup_d"))
            p.dma("sp", aup[64:128, :], dv(self.a_up.ap()[l], "aup_d"))
            p.dma("sp", gup[:, :], dv(self.g_up.ap()[l], "gup_d"))
            b1 = cs[:, C_B1:C_B1 + 128]
            b64 = cs[:, C_B64:C_B64 + 128]
            rst = cs[:, C_RST:C_RST + TT]
            zd = self.z_d.ap()
            col = lambda c: pp[:, c:c + 1]

            def load_halo(tile, row0, nrows, t0, j, halo, first, gkeys, prow=0):
                rows = slice(prow, prow + nrows)
                if first:
                    if halo > 0:
                        p.memset("pool", tile[rows, 0:halo], 0.0)
                    p.dma("sp", tile[rows, halo:halo + TT], dv(zd[row0:row0 + nrows, t0:t0 + TT], *[(("z",) + (g, j)) for g in gkeys]))
                else:
                    p.dma("sp", tile[rows, 0:halo + TT], dv(zd[row0:row0 + nrows, t0 - halo:t0 + TT],
                                                           *([(("z",) + (g, j)) for g in gkeys] + [(("z",) + (g, j - 1)) for g in gkeys])))

            def zkey(row0):
                g = row0 // 128
                return [g - (g % 2), g - (g % 2) + 1]

            def l2rn(x, tmp, tmp2, bank, eps, blk):
                p.act(tmp[:, 0:TT], x, AF.Square)
                p.mm(bk(bank), blk, tmp[:, 0:TT])
                p.act(tmp2[:, 0:TT], bk(bank), AF.Sqrt, bias=eps)
                p.recip(tmp2[:, 0:TT], tmp2[:, 0:TT])

            for sq in range(self.mix_nseq):
                for hp in range(2):
                    p.memset("pool", Hst[hp][:], 0.0)
                for jt in range(self.mix_tiles if "rwkv" in self.mix_parts else 0):
                    j = sq * TPS + jt
                    t0 = j * TT
                    first = (jt == 0)
                    Z = W[0:8]
                    XS = W[8:16]
                    for role in range(8):
                        load_halo(Z[role], role * 128, 128, t0, j, 1, first, zkey(role * 128))
                        eng = "dve" if role % 2 == 0 else "pool"
                        p.tt(eng, W[16][:, 0:TT], Z[role][:, 0:TT], Z[role][:, 1:TT + 1], ALU.subtract)
                        p.stt(eng, XS[role][:, 0:TT], W[16][:, 0:TT], col(P_MU + role), Z[role][:, 1:TT + 1], ALU.mult, ALU.add)
                    xr, xk, xv, xdd, xdg = XS[0:2], XS[2:4], XS[4:6], XS[6], XS[7]
                    p.act(xdd[0:64, 0:TT], xdd[0:64, 0:TT], AF.Tanh)
                    p.act(xdg[:, 0:TT], xdg[:, 0:TT], AF.Sigmoid)
                    lw, iclr, gate, kk = W[0:2], W[2:4], W[4:6], W[6:8]
                    cum, Pin, Pex, Pinv = W[17:19], W[19:21], W[21:23], W[23:25]
                    bT, kT, bon = Pex, Pinv, lw
                    yo = cum
                    tmp = W[16]
                    tmp2 = W[25]
                    for hp in range(2):
                        hsl = slice(hp * 128, hp * 128 + 128)
                        p.mm(bk(0), wup[0:64, hsl], xdd[0:64, 0:TT])
                        p.act(lw[hp][:, 0:TT], bk(0), AF.Sigmoid, bias=col(P_W0 + hp))
                        p.ts("dve", lw[hp][:, 0:TT], lw[hp][:, 0:TT], DECAY_C, ALU.mult)
                        p.mm(bk(1), aup[64:128, hsl], xdd[64:128, 0:TT])
                        p.act(iclr[hp][:, 0:TT], bk(1), AF.Sigmoid, bias=col(P_A0 + hp))
                        p.mm(bk(0), gup[:, hsl], xdg[:, 0:TT])
                        p.cp("act", gate[hp][:, 0:TT], bk(0))
                        p.ts("dve", kk[hp][:, 0:TT], xk[hp][:, 0:TT], col(P_KK + hp), ALU.mult)
                        l2rn(kk[hp][:, 0:TT], tmp, tmp2, 1, 1e-6, b1)
                        p.tt("dve", kk[hp][:, 0:TT], kk[hp][:, 0:TT], tmp2[:, 0:TT], ALU.mult)
                        p.ts("pool", tmp[:, 0:TT], iclr[hp][:, 0:TT], -1.0, ALU.add, col(P_KA + hp), ALU.mult)
                        p.stt("pool", xk[hp][:, 0:TT], tmp[:, 0:TT], 1.0, xk[hp][:, 0:TT], ALU.add, ALU.mult)
                        p.scan(cum[hp][:, 0:TT], rst, lw[hp][:, 0:TT])
                        p.act(Pin[hp][:, 0:TT], cum[hp][:, 0:TT], AF.Exp)
                        p.act(Pinv[hp][:, 0:TT], cum[hp][:, 0:TT], AF.Exp, scale=-1.0)
                        p.tt("dve", tmp[:, 0:TT], cum[hp][:, 0:TT], lw[hp][:, 0:TT], ALU.subtract)
                        p.act(Pex[hp][:, 0:TT], tmp[:, 0:TT], AF.Exp)
                        a3 = arT[hp][:]
                        v3 = lambda t: t[:, 0:TT].rr("p (c t) -> p c t", t=64)
                        p.stt("dve", a3[:, :, 0:64], v3(kk[hp]), -1.0, v3(Pex[hp]), ALU.mult, ALU.mult)
                        p.tt("pool", a3[:, :, 64:128], v3(xr[hp]), v3(Pin[hp]), ALU.mult)
                        p.tt("dve", tmp[:, 0:TT], kk[hp][:, 0:TT], iclr[hp][:, 0:TT], ALU.mult)
                        p.tt("dve", bT[hp][:, 0:TT], tmp[:, 0:TT], Pinv[hp][:, 0:TT], ALU.mult)
                        p.tt("pool", kT[hp][:, 0:TT], xk[hp][:, 0:TT], Pinv[hp][:, 0:TT], ALU.mult)
                        p.stt("dve", tmp[:, 0:TT], xr[hp][:, 0:TT], col(P_RK + hp), xk[hp][:, 0:TT], ALU.mult, ALU.mult)
                        p.mm(bk(1), b1, tmp[:, 0:TT])
                        p.tt("dve", bon[hp][:, 0:TT], bk(1), xv[hp][:, 0:TT], ALU.mult)
                    pc = lambda hp, c: Pin[hp][:, c * 64 + 63: c * 64 + 64]
                    self.dplr_tile(s, banks, arT, [t[:, 0:TT] for t in bT], [t[:, 0:TT] for t in kT],
                                   [t[:, 0:TT] for t in xv], pc, Hst, [t[:, 0:TT] for t in yo])
                    for hp in range(2):
                        y = yo[hp][:, 0:TT]
                        p.mm(bk(0), b64, y)
                        p.tt("dve", y, y, bk(0), ALU.subtract)
                        l2rn(y, tmp, tmp2, 1, 64e-5, b64)
                        p.tt("dve", y, y, tmp2[:, 0:TT], ALU.mult)
                        p.ts("dve", y, y, col(P_LXW + hp), ALU.mult, col(P_LXB + hp), ALU.add)
                        p.tt("pool", y, y, bon[hp][:, 0:TT], ALU.add)
                        p.tt("dve", yb16[hp][:], y, gate[hp][:, 0:TT], ALU.mult)
                        p.dma("pool", dv(self.y_d.ap()[hp * 128:(hp + 1) * 128, t0:t0 + TT], ("y", hp, j)), yb16[hp][:])
                for hp in range(2):
                    p.memset("pool", Hst[hp][:], 0.0)
                for jt in range(self.mix_tiles if "gdn" in self.mix_parts else 0):
                    j = sq * TPS + jt
                    t0 = j * TT
                    first = (jt == 0)
                    Z = W[0:6]
                    CV = W[6:12]
                    gt = W[12:14]
                    for i in range(6):
                        r0 = 1024 + i * 128
                        load_halo(Z[i], r0, 128, t0, j, 3, first, zkey(r0))
                        eng = "dve" if i % 2 == 0 else "pool"
                        cw = lambda k: col(P_CONV + i * 4 + k)
                        o = CV[i][:, 0:TT]
                        p.ts(eng, o, Z[i][:, 0:TT], cw(0), ALU.mult)
                        for k in range(1, 4):
                            p.stt(eng, o, Z[i][:, k:k + TT], cw(k), o, ALU.mult, ALU.add)
                        p.act(o, o, AF.Silu)
                    for hp in range(2):
                        r0 = 1792 + hp * 128
                        load_halo(gt[hp], r0, 128, t0, j, 0, True, zkey(r0))
                        p.act(gt[hp][:, 0:TT], gt[hp][:, 0:TT], AF.Silu)
                    bl, al = sm[0], sm[1]
                    p.dma("sp", bl[:], dv(zd[2048:2052, t0:t0 + TT], ("z", 16, j), ("z", 17, j)))
                    p.dma("sp", al[:], dv(zd[2052:2056, t0:t0 + TT], ("z", 16, j), ("z", 17, j)))
                    beta, xx, ax, ee, gg, gam, E1 = sm[0:7]
                    nb, BE1n, Ff, BF = xx, ax, ee, gg
                    p.act(beta[:], bl[:], AF.Sigmoid)
                    p.ts("dve", xx[:], al[:], pp[0:4, P_DTB:P_DTB + 1], ALU.add)
                    p.act(ax[:], xx[:], AF.Abs)
                    p.act(ee[:], ax[:], AF.Exp, scale=-1.0)
                    p.act(ee[:], ee[:], AF.Ln, bias=1.0)
                    p.stt("dve", gg[:], xx[:], 0.0, ee[:], ALU.max, ALU.add)
                    p.ts("dve", gg[:], gg[:], self.der[0:4, 1:2], ALU.mult)
                    p.scan(gam[:], cs[0:4, C_RST:C_RST + TT], gg[:])
                    p.act(E1[:], gam[:], AF.Exp)
                    p.ts("dve", nb[:], beta[:], -1.0, ALU.mult)
                    p.tt("dve", BE1n[:], nb[:], E1[:], ALU.mult)
                    g3 = gam[:].rr("p (c t) -> p c t", t=64)
                    gend = View(g3.ap[:, :, 63:64].broadcast_to([4, NCH, 64]), g3.keys)
                    p.tt("dve", Ff[:].rr("p (c t) -> p c t", t=64), gend, g3, ALU.subtract)
                    p.act(Ff[:], Ff[:], AF.Exp)
                    p.tt("dve", BF[:], beta[:], Ff[:], ALU.mult)
                    cq, ck, cv = CV[0:2], CV[2:4], CV[4:6]
                    bT, kT, yo = W[14:16], W[16:18], W[18:20]
                    tmp, tmp2 = W[20], W[21]
                    pct = W[22]
                    kG = W[23:25]
                    for hp in range(2):
                        l2rn(cq[hp][:, 0:TT], tmp, tmp2, 0, 1e-6, b1)
                        p.stt("dve", cq[hp][:, 0:TT], cq[hp][:, 0:TT], 0.125, tmp2[:, 0:TT], ALU.mult, ALU.mult)
                        l2rn(ck[hp][:, 0:TT], tmp, tmp2, 1, 1e-6, b1)
                        p.tt("dve", ck[hp][:, 0:TT], ck[hp][:, 0:TT], tmp2[:, 0:TT], ALU.mult)
                        a3 = arT[hp][:]
                        g3a = arG[hp][:]
                        v3 = lambda t: t[:, 0:TT].rr("p (c t) -> p c t", t=64)
                        pv3 = lambda b: bk(b).rr("p (c t) -> p c t", t=64)

                        def bcast(bank, fac):
                            for hh in range(2):
                                h = hp * 2 + hh
                                p.mm(bks(bank, (slice(hh * 64, hh * 64 + 64), slice(0, TT))), cs[0:4, C_SEL + h * 64: C_SEL + h * 64 + 64],
                                     fac[:], inc=(hh == 1))
                        bcast(0, nb)
                        p.tt("dve", g3a[:, :, 0:64], v3(ck[hp]), pv3(0), ALU.mult)
                        p.cp("pool", g3a[:, :, 64:128], v3(cq[hp]))
                        bcast(1, BE1n)
                        p.tt("dve", a3[:, :, 0:64], v3(ck[hp]), pv3(1), ALU.mult)
                        bcast(0, E1)
                        p.tt("dve", a3[:, :, 64:128], v3(cq[hp]), pv3(0), ALU.mult)
                        p.cp("act", pct[:, hp * 8:hp * 8 + 8], bk(0).rr("p (c t) -> p c t", t=64)[:, :, 63])
                        bcast(1, beta)
                        p.tt("dve", kG[hp][:, 0:TT], ck[hp][:, 0:TT], bk(1), ALU.mult)
                        bcast(0, Ff)
                        p.tt("dve", bT[hp][:, 0:TT], ck[hp][:, 0:TT], bk(0), ALU.mult)
                        bcast(1, BF)
                        p.tt("dve", kT[hp][:, 0:TT], ck[hp][:, 0:TT], bk(1), ALU.mult)
                    pc = lambda hp, c: pct[:, hp * 8 + c: hp * 8 + c + 1]
                    self.dplr_tile(s, banks, arT, [t[:, 0:TT] for t in bT], [t[:, 0:TT] for t in kT],
                                   [t[:, 0:TT] for t in cv], pc, Hst, [t[:, 0:TT] for t in yo],
                                   arG=arG, bG=[t[:, 0:TT] for t in ck], kG=[t[:, 0:TT] for t in kG], gam=gam)
                    for hp in range(2):
                        y = yo[hp][:, 0:TT]
                        l2rn(y, tmp, tmp2, 0, 1e-6, b64)
                        p.stt("dve", y, y, col(P_GNW), tmp2[:, 0:TT], ALU.mult, ALU.mult)
                        p.tt("dve", yb16[hp][:], y, gt[hp][:, 0:TT], ALU.mult)
                        p.dma("pool", dv(self.y_d.ap()[256 + hp * 128:256 + (hp + 1) * 128, t0:t0 + TT], ("y", 2 + hp, j)), yb16[hp][:])
                if "attn" in self.mix_parts:
                    self.attn_seq(sq, W, banks, yb16, st)

    def attn_seq(self, sq, W, banks, yb16, st):
        p = self.p
        cs = self.cs
        pp = self.ppt
        bk = lambda i: dv(banks[i][:, :], f"pb{i}")
        bks = lambda i, sl: dv(banks[i][sl], f"pb{i}")
        zd = self.z_d.ap()
        ident = cs[:, C_ID:C_ID + 128]
        b64 = cs[:, C_B64:C_B64 + 128]
        if not hasattr(self, "_attn_tiles"):
            v1 = self.sb(st, [128, 5, 2, 65], F32, "v1")
            kt = self.sb(st, [128, 128 + TT], F32, "kt")
            osb = self.sb(st, [128, 512], F32, "osb")
            den = self.sb(st, [128, 8], F32, "den")
            ya = [self.sb(st, [128, TT], BF16, "ya") for _ in range(4)]
            self._attn_tiles = (v1, kt, osb, den, ya)
        v1, kt, osb, den, ya = self._attn_tiles
        for jt in range(self.mix_tiles):
            j = sq * TPS + jt
            t0 = j * TT
            first = (jt == 0)
            Q = W[0:4]
            tmp, tmp2 = W[4], W[5]
            E = W[6:10]
            p.memset("pool", v1[:, :, :, 64:65], 1.0)
            for t in range(4):
                for g in range(2):
                    h = g * 4 + t
                    r0 = 2056 + h * 64
                    gk = r0 // 128
                    gk2 = (r0 + 63) // 128
                    keys = set()
                    for gg in (gk, gk2):
                        keys.add(("z", gg - (gg % 2), j))
                        keys.add(("z", gg - (gg % 2) + 1, j))
                    p.dma("sp", Q[t][g * 64:g * 64 + 64, 0:TT], dv(zd[r0:r0 + 64, t0:t0 + TT], *keys))
            kkeys = [("z", 20, j), ("z", 21, j)]
            if first:
                p.memset("pool", kt[:, 0:128], 0.0)
                p.dma("sp", kt[:, 128:128 + TT], dv(zd[2568:2696, t0:t0 + TT], *kkeys))
            else:
                p.dma("sp", kt[:, 0:128 + TT], dv(zd[2568:2696, t0 - 128:t0 + TT], *(kkeys + [("z", 20, j - 1), ("z", 21, j - 1)])))
            vt = self.vtm_d.ap()
            if not first:
                p.dma("sp", v1[:, 0, :, 0:64], dv(vt[t0 - 128:t0, :].rearrange("p (g d) -> p g d", g=2), ("vtm", j - 1)))
            for q in range(4):
                p.dma("sp", v1[:, 1 + q, :, 0:64], dv(vt[t0 + q * 128:t0 + (q + 1) * 128, :].rearrange("p (g d) -> p g d", g=2), ("vtm", j)))
            for t in range(4):
                q = Q[t][:, 0:TT]
                p.act(tmp[:, 0:TT], q, AF.Square)
                p.mm(bk(0), b64, tmp[:, 0:TT])
                p.act(tmp2[:, 0:TT], bk(0), AF.Sqrt, bias=1e-6)
                p.recip(tmp2[:, 0:TT], tmp2[:, 0:TT])
                p.stt("dve", q, q, self.der[:, 0:1], tmp2[:, 0:TT], ALU.mult, ALU.mult)
            for part in range(2):
                if part == 0:
                    if first:
                        continue
                    ks = slice(0, 128)
                else:
                    ks = slice(128, 128 + TT)
                n = ks.stop - ks.start
                k = kt[:, ks]
                p.act(tmp[:, 0:n], k, AF.Square)
                p.mm(bks(1, (slice(0, 128), slice(0, n))), b64, tmp[:, 0:n])
                p.act(tmp2[:, 0:n], bks(1, (slice(0, 128), slice(0, n))), AF.Sqrt, bias=1e-6)
                p.recip(tmp2[:, 0:n], tmp2[:, 0:n])
                p.stt("dve", k, k, pp[:, P_KNW:P_KNW + 1], tmp2[:, 0:n], ALU.mult, ALU.mult)
            for n in range(4 if CUT > 1 else 0):
                qs = slice(n * 128, (n + 1) * 128)
                noprev = first and n == 0
                for g in range(2):
                    gs = slice(g * 64, g * 64 + 64)
                    for kind in range(2):
                        if kind == 0 and noprev:
                            continue
                        kcols = slice(n * 128 + kind * 128, n * 128 + kind * 128 + 128)
                        b = 2 + kind
                        for t in range(4):
                            p.mm(bks(b, (slice(0, 128), slice(t * 128, t * 128 + 128))), kt[gs, kcols], Q[t][gs, qs], inc=(t == 3))
                        Ek = E[kind * 2 + (g % 2)]
                        bo = (kind * 8 + g * 4) * 128
                        p.tt("dve", Ek[:, 0:TT], bk(b), self.bm[:, bo:bo + 512], ALU.add)
                        p.act(Ek[:, 0:TT], Ek[:, 0:TT], AF.Exp)
                    ob = 4 + g
                    if CUT <= 2:
                        continue
                    for t in range(4):
                        o = bks(ob, (slice(0, 128), slice(t * 65, t * 65 + 65)))
                        if not noprev:
                            p.mm(o, E[0 + g][:, t * 128:t * 128 + 128], v1[:, n, g, :], start=True, stop=False, inc=False)
                        p.mm(o, E[2 + g][:, t * 128:t * 128 + 128], v1[:, n + 1, g, :], start=noprev, stop=True, inc=(t == 3))
                    if CUT <= 3:
                        continue
                    o3 = bks(ob, (slice(0, 128), slice(0, 260))).rr("p (t e) -> p t e", e=65)
                    p.tt("dve", den[:, g * 4:g * 4 + 4], o3[:, :, 64], self.der[:, 2 + g * 4: 2 + g * 4 + 4], ALU.add)
                    p.recip(den[:, g * 4:g * 4 + 4], den[:, g * 4:g * 4 + 4])
                    for t in range(4):
                        h = g * 4 + t
                        if t % 2 == 0:
                            p.ts("dve", osb[:, h * 64:h * 64 + 64], o3[:, t, 0:64], den[:, h:h + 1], ALU.mult)
                        else:
                            p.act(osb[:, h * 64:h * 64 + 64], o3[:, t, 0:64], AF.Copy, scale=den[:, h:h + 1])
                TRV = int(os.environ.get("TRV", "0"))
                for hp4 in range(4 if CUT > 4 else 0):
                    tb = (6 if TRV != 1 else 0) + hp4 // 2
                    src_t = osb if TRV != 2 else Q[0]
                    p.tr(bks(tb, (slice(0, 128), slice((hp4 % 2) * 128, (hp4 % 2) * 128 + 128))),
                         src_t[:, hp4 * 128:(hp4 + 1) * 128], ident, inc=(hp4 % 2 == 1) or TRV == 3)
                for hh in range(2 if (CUT > 4 and CUT != 7) else 0):
                    for q2 in range(2):
                        hp4 = hh * 2 + q2
                        src = bks(6 + hh, (slice(0, 128), slice(q2 * 128, q2 * 128 + 128)))
                        if q2 == 0:
                            p.cp("act", ya[hp4][:, qs], src)
                        else:
                            p.cp("dve", ya[hp4][:, qs], src)
            for hp4 in range(4 if CUT not in (6, 7) else 0):
                p.dma("pool", dv(self.y_d.ap()[512 + hp4 * 128:512 + (hp4 + 1) * 128, t0:t0 + TT], ("y", 4 + hp4, j)), ya[hp4][:])

    def phase_outproj(self, l, xsrc, xdst):
        p = self.p
        if hasattr(self, "_attn_tiles"):
            del self._attn_tiles
        with contextlib.ExitStack() as st:
            wb = self.sb(st, [128, 8, D], BF16, "woutb")
            for c in range(8):
                p.dma("pool", wb[:, c, :], dv(self.w_out.ap()[l, c * 128:(c + 1) * 128, :], "w_out_d"))
            xts = [self.sb(st, [128, 8, TT], F32, "xt") for _ in range(2)]
            yts = [self.sb(st, [128, 8, TT], BF16, "yt") for _ in range(2)]
            banks = self.ps(st, 8)
            xs_v = xsrc.ap().rearrange("(c p) t -> p c t", p=128)
            xd_v = xdst.ap().rearrange("(c p) t -> p c t", p=128)
            yv = self.y_d.ap().rearrange("(c p) t -> p c t", p=128)
            for j in range(NTILE):
                t0 = j * TT
                xt, yt = xts[j % 2], yts[j % 2]
                for hh in range(2):
                    p.dma("sp", xt[:, hh * 4:(hh + 1) * 4, :], dv(xs_v[:, hh * 4:(hh + 1) * 4, t0:t0 + TT], ("x", id(xsrc), j)))
                p.dma("sp", yt[:], dv(yv[:, :, t0:t0 + TT], *[("y", c, j) for c in range(8)]))
                for m in range(8):
                    pv = dv(banks[m][:, :], f"pb{m}")
                    for c in range(8):
                        p.mm(pv, wb[:, c, m * 128:(m + 1) * 128], yt[:, c, :], start=(c == 0), stop=(c == 7), inc=(c == 7))
                    p.tt("dve", xt[:, m, :], xt[:, m, :], pv, ALU.add)
                for hh in range(2):
                    p.dma("pool", dv(xd_v[:, hh * 4:(hh + 1) * 4, t0:t0 + TT], ("x", id(xdst), j)), xt[:, hh * 4:(hh + 1) * 4, :])

    def phase_ffn(self, l, xsrc, xdst):
        p = self.p
        with contextlib.ExitStack() as st:
            w1 = self.sb(st, [128, 8, DFF], BF16, "w1b")
            w2 = self.sb(st, [128, 32, D], BF16, "w2b")
            for c in range(8):
                p.dma("pool", w1[:, c, :], dv(self.w_ff1.ap()[l, c * 128:(c + 1) * 128, :], "w1_d"))
            w2v = self.w_ff2.ap()[l].rearrange("(c p) n -> p c n", p=128)
            for c4 in range(8):
                p.dma("pool", w2[:, c4 * 4:(c4 + 1) * 4, :], dv(w2v[:, c4 * 4:(c4 + 1) * 4, :], "w2_d"))
            xt = self.sb(st, [128, 8, TT], F32, "xt")
            ht = self.sb(st, [128, 8, TT], BF16, "ht")
            h1 = self.sb(st, [128, 32, TT], BF16, "h1")
            sq2 = [self.sb(st, [128, TT], F32, "sq") for _ in range(2)]
            rstd = self.sb(st, [128, TT], F32, "rstd")
            banks = self.ps(st, 8)
            xs_v = xsrc.ap().rearrange("(c p) t -> p c t", p=128)
            xd_v = xdst.ap().rearrange("(c p) t -> p c t", p=128)
            for j in range(NTILE):
                t0 = j * TT
                for hh in range(2):
                    p.dma("sp", xt[:, hh * 4:(hh + 1) * 4, :], dv(xs_v[:, hh * 4:(hh + 1) * 4, t0:t0 + TT], ("x", id(xsrc), j)))
                self.rms_tile(xt, ht, P_LN2, sq2, rstd, banks[7], "pb7")
                for f in range(32):
                    b = f % 6
                    pv = dv(banks[b][:, :], f"pb{b}")
                    for c in range(8):
                        p.mm(pv, w1[:, c, f * 128:(f + 1) * 128], ht[:, c, :], start=(c == 0), stop=(c == 7), inc=(c == 7))
                    s = sq2[f % 2]
                    p.act(s[:], pv, AF.Relu)
                    p.tt("dve" if f % 2 == 0 else "pool", h1[:, f, :], s[:], s[:], ALU.mult)
                for m in range(8):
                    b = m % 6
                    pv = dv(banks[b][:, :], f"pb{b}")
                    for f in range(32):
                        p.mm(pv, w2[:, f, m * 128:(m + 1) * 128], h1[:, f, :], start=(f == 0), stop=(f == 31), inc=(f == 31))
                    p.tt("dve", xt[:, m, :], xt[:, m, :], pv, ALU.add)
                for hh in range(2):
                    p.dma("pool", dv(xd_v[:, hh * 4:(hh + 1) * 4, t0:t0 + TT], ("x", id(xdst), j)), xt[:, hh * 4:(hh + 1) * 4, :])


def _t5_bucket_np(dist):
    max_exact = 16
    nf = np.maximum(dist, max_exact).astype(np.float32)
    large = max_exact + (np.log(nf / max_exact) / np.float32(np.log(128 / max_exact)) * (32 - max_exact)).astype(np.int32)
    large = np.minimum(large, 31)
    return np.where(dist < max_exact, dist, large)


def _consts():
    c = np.zeros((128, NCST), np.float32)
    c[:, C_ID:C_ID + 128] = np.eye(128, dtype=np.float32)
    blk = np.kron(np.eye(2, dtype=np.float32), np.ones((64, 64), np.float32))
    c[:, C_B1:C_B1 + 128] = blk
    c[:, C_B64:C_B64 + 128] = blk / 64.0
    c[:, C_OD:C_OD + 128] = 1.0 / 1024.0
    rst = np.ones((128, TT), np.float32)
    rst[:, ::64] = 0.0
    c[:, C_RST:C_RST + TT] = rst
    s_ = np.arange(64)[:, None]
    t_ = np.arange(64)[None, :]
    strict = (s_ < t_).astype(np.float32)
    incl = (s_ <= t_).astype(np.float32)
    mt = np.concatenate([strict, incl, strict, incl], 1)
    c[0:64, C_MT4:C_MT4 + 1024] = np.tile(mt, (1, 4))
    msl = (t_ < s_).astype(np.float32)
    c[0:64, C_MSL4:C_MSL4 + 256] = np.tile(msl, (1, 4))
    c[0:64, C_I64:C_I64 + 256] = np.tile(np.eye(64, dtype=np.float32), (1, 4))
    for h in range(4):
        c[h, C_SEL + h * 64:C_SEL + (h + 1) * 64] = 1.0
    c[0:64, C_MNI:C_MNI + 64] = np.where(s_ <= t_, 0.0, NEG)
    jj = np.arange(128)[:, None]
    ii = np.arange(128)[None, :]
    c[:, C_BM:C_BM + 128] = np.where(jj > ii, 0.0, NEG)
    c[:, C_BM + 128:C_BM + 256] = np.where(jj <= ii, 0.0, NEG)
    return c


def _bias_band(rel_bias):
    i = np.arange(128)[None, :]
    j = np.arange(128)[:, None]
    out = np.zeros((128, 2, 8, 128), np.float32)
    d_prev = np.maximum(i + 128 - j, 0)
    d_cur = np.maximum(i - j, 0)
    for kind, dd in enumerate((d_prev, d_cur)):
        b = _t5_bucket_np(dd)
        out[:, kind, :, :] = np.transpose(rel_bias[b], (0, 2, 1))
    return out.reshape(128, 2 * 8 * 128)


def _pack_params(inp, l):
    pp = np.zeros((128, NPP), np.float32)
    f = lambda a, n: np.asarray(a, np.float32).reshape(n, 128).T
    pp[:, P_LN1:P_LN1 + 8] = f(inp["ln1_w"][l], 8)
    pp[:, P_LN2:P_LN2 + 8] = f(inp["ln2_w"][l], 8)
    pp[:, P_MU:P_MU + 8] = f(inp["rwkv_mu"][l], 8)
    pp[:, P_W0:P_W0 + 2] = f(inp["rwkv_w0"][l], 2)
    pp[:, P_A0:P_A0 + 2] = f(inp["rwkv_a0"][l], 2)
    pp[:, P_KK:P_KK + 2] = f(inp["rwkv_k_k"][l], 2)
    pp[:, P_KA:P_KA + 2] = f(inp["rwkv_k_a"][l], 2)
    pp[:, P_RK:P_RK + 2] = f(inp["rwkv_r_k"][l].reshape(256), 2)
    pp[:, P_LXW:P_LXW + 2] = f(inp["rwkv_lnx_w"][l], 2)
    pp[:, P_LXB:P_LXB + 2] = f(inp["rwkv_lnx_b"][l], 2)
    cw = np.asarray(inp["gdn_conv_w"][l], np.float32)
    for i in range(6):
        pp[:, P_CONV + i * 4:P_CONV + i * 4 + 4] = cw[:, i * 128:(i + 1) * 128].T
    pp[:, P_GNW] = np.tile(inp["gdn_norm_w"][l], 2)
    pp[:, P_QNW] = np.tile(inp["attn_q_norm_w"][l], 2)
    pp[:, P_KNW] = np.tile(inp["attn_k_norm_w"][l], 2)
    pp[0:4, P_ALOG] = inp["gdn_a_log"][l]
    pp[0:4, P_DTB] = inp["gdn_dt_bias"][l]
    pp[:, P_SINK:P_SINK + 8] = np.asarray(inp["attn_sinks"][l], np.float32)[None, :]
    return pp


_NC_CACHE = {}
LAYERS_PER_LAUNCH = 4


def _get_nc(nl):
    if nl not in _NC_CACHE:
        _NC_CACHE[nl] = Builder(nl).build()
    return _NC_CACHE[nl]


def kernel(**inp):
    inp = {k: np.asarray(v) for k, v in inp.items()}
    x = inp["x"].astype(np.float32)
    depth = inp["w_in"].shape[0]
    cst = _consts()
    bband = _bias_band(inp["rel_bias"].astype(np.float32))
    xT = [np.ascontiguousarray(x[2 * c:2 * c + 2].reshape(NTOK, D).T) for c in range(NCORES)]
    nl = LAYERS_PER_LAUNCH
    for l0 in range(0, depth, nl):
        nc = _get_nc(nl)
        sl = slice(l0, l0 + nl)
        pp = np.stack([_pack_params(inp, l) for l in range(l0, l0 + nl)])
        shared = {
            "w_in": np.ascontiguousarray(inp["w_in"][sl], np.float32),
            "w_out": np.ascontiguousarray(inp["w_out"][sl], np.float32),
            "w_ff1": np.ascontiguousarray(inp["w_ff1"][sl], np.float32),
            "w_ff2": np.ascontiguousarray(inp["w_ff2"][sl], np.float32),
            "w_up": np.ascontiguousarray(inp["rwkv_w_up"][sl], np.float32),
            "a_up": np.ascontiguousarray(inp["rwkv_a_up"][sl], np.float32),
            "g_up": np.ascontiguousarray(inp["rwkv_g_up"][sl], np.float32),
            "pp": pp, "cst": cst, "bband": bband,
        }
        in_maps = [dict(shared, xT=xT[c]) for c in range(NCORES)]
        res = run_bass_kernel_spmd(nc, in_maps, core_ids=list(range(NCORES)))
        xT = [np.ascontiguousarray(res.results[c]["outT"]) for c in range(NCORES)]
    out = np.stack([xT[c].T.reshape(NSEQ, SEQ, D) for c in range(NCORES)]).reshape(16, SEQ, D)
    return out.astype(np.float32)
```
